# Optimizing a Trainium2 kernel written in Bass

```python
import jax
import jax.numpy as jnp
from jax import lax
import numpy as np

D_MODEL = 1024
BATCH = 4
SEQ = 4096
DEPTH = 4

RW_HEADS = 8
RW_HEAD = 64
RW_DIM = RW_HEADS * RW_HEAD
RW_LORA_W = 64
RW_LORA_A = 64
RW_LORA_G = 128
RW_SPLITS = [RW_DIM, 2 * RW_DIM, 3 * RW_DIM, 3 * RW_DIM + RW_LORA_W,
             3 * RW_DIM + RW_LORA_W + RW_LORA_A]
SG_GROUPS = 4
SG_CHUNK = 128
SG_GROUP_DIM = 128
SG_DIM = SG_GROUPS * SG_GROUP_DIM
AB_SHIFT_DIM = 3 * RW_DIM + RW_LORA_W + RW_LORA_A + RW_LORA_G
AB_IN_DIM = AB_SHIFT_DIM + 2 * SG_DIM
AB_MIX_DIM = RW_DIM + SG_DIM

MLA_HEADS = 16
MLA_Q_RANK = 256
MLA_KV_RANK = 256
MLA_NOPE = 64
MLA_ROPE = 32
MLA_V = 64
MLA_QK = MLA_NOPE + MLA_ROPE
MLA_IN_DIM = MLA_Q_RANK + MLA_KV_RANK + MLA_ROPE
MLA_MIX_DIM = MLA_HEADS * MLA_V
ROPE_THETA = 10000.0
Q_BLOCK = 128

MEM_LEN = 256
XA_HEADS = 4
XA_HEAD = D_MODEL // XA_HEADS

MOE_GROUPS = 4
MOE_PER_GROUP = 8
MOE_EXPERTS = MOE_GROUPS * MOE_PER_GROUP
MOE_TOPK = 2
MOE_FF = 512
MOE_BLOCK = 128

DN_ALPHA = (2 * DEPTH) ** 0.25
DN_BETA = (8 * DEPTH) ** -0.25
LN_EPS = 1e-5
RMS_EPS = 1e-6
RW_GN_EPS = 64e-5
N_EVEN = (DEPTH + 1) // 2
N_ODD = DEPTH // 2

kernel_name = 'hybrid_rwkv7_sgmlp_mla_hmoe_deepnorm'


def _standardize(x, eps):
    xf = x.astype(jnp.float32)
    mu = jnp.mean(xf, -1, keepdims=True)
    var = jnp.mean(jnp.square(xf - mu), -1, keepdims=True)
    return ((xf - mu) * lax.rsqrt(var + eps)).astype(x.dtype)


def layer_norm(x, g, b, eps=LN_EPS):
    return _standardize(x, eps) * g + b


def rms_norm(x, g, eps=RMS_EPS):
    xf = x.astype(jnp.float32)
    return (xf * lax.rsqrt(jnp.mean(xf * xf, -1, keepdims=True) + eps)).astype(x.dtype) * g


def token_shift(p):
    return jnp.pad(p, ((0, 0), (1, 0), (0, 0)))[:, :-1]


def rwkv7_recurrence(r, w, k, v, kk, a):
    B, S, H, N = r.shape

    def step(state, inp):
        r_t, w_t, k_t, v_t, kk_t, a_t = inp
        sa = jnp.einsum('bhvk,bhk->bhv', state, -kk_t)
        state = (state * w_t[:, :, None, :]
                 + sa[..., None] * (kk_t * a_t)[:, :, None, :]
                 + v_t[..., None] * k_t[:, :, None, :])
        return state, jnp.einsum('bhvk,bhk->bhv', state, r_t)

    xs = tuple(jnp.moveaxis(t.astype(jnp.float32), 1, 0) for t in (r, w, k, v, kk, a))
    state0 = jnp.zeros((B, H, N, N), jnp.float32)
    _, y = lax.scan(step, state0, xs)
    return jnp.moveaxis(y, 0, 1)


def rwkv7_mix(p, w0, w2, a0, a2, g2, k_k, k_a, r_k, gn_g, gn_b):
    B, S, _ = p.shape
    heads = lambda t: t.reshape(B, S, RW_HEADS, RW_HEAD)
    r, k, v, w_lo, a_lo, g_lo = jnp.split(p, RW_SPLITS, axis=-1)
    w_log = -jax.nn.softplus(-(w0 + jnp.tanh(w_lo) @ w2)) - 0.5
    decay = jnp.exp(-jnp.exp(w_log.astype(jnp.float32)))
    a = jax.nn.sigmoid(a0 + a_lo @ a2)
    g = jax.nn.sigmoid(g_lo) @ g2
    kk = heads(k * k_k).astype(jnp.float32)
    kk = kk / jnp.maximum(jnp.sqrt(jnp.sum(kk * kk, -1, keepdims=True)), 1e-12)
    k = k * (1.0 + (a - 1.0) * k_a)
    r_h, k_h, v_h = heads(r), heads(k), heads(v)
    y = rwkv7_recurrence(r_h, heads(decay), k_h, v_h, kk, heads(a))
    y = _standardize(y, RW_GN_EPS).astype(p.dtype).reshape(B, S, RW_DIM) * gn_g + gn_b
    bonus = jnp.sum(r_h * k_h * r_k, -1, keepdims=True) * v_h
    return (y + bonus.reshape(B, S, RW_DIM)) * g


def spatial_gating(pu, pv, ln_g, ln_b, ws, bs):
    B, S, _ = pu.shape
    n_chunk = S // SG_CHUNK
    u = jax.nn.gelu(pu)
    z = jax.nn.gelu(pv).reshape(B, n_chunk, SG_CHUNK, SG_GROUPS, SG_GROUP_DIM)
    z = layer_norm(z, ln_g, ln_b)
    causal = jnp.tril(jnp.ones((SG_CHUNK, SG_CHUNK), dtype=bool))
    wm = jnp.where(causal, ws, jnp.zeros_like(ws))
    zs = jnp.einsum('gts,bcsgd->bctgd', wm, z) + bs.T[:, :, None]
    return u * zs.reshape(B, S, SG_DIM)


def mixer_ab(x, w_in, mu, w0, w2, a0, a2, g2, k_k, k_a, r_k, gn_g, gn_b,
             ln_g, ln_b, ws, bs, w_out):
    p = x @ w_in
    ps = p[..., :AB_SHIFT_DIM]
    pu = p[..., AB_SHIFT_DIM:AB_SHIFT_DIM + SG_DIM]
    pv = p[..., AB_SHIFT_DIM + SG_DIM:]
    ps = ps + (token_shift(ps) - ps) * mu
    ya = rwkv7_mix(ps, w0, w2, a0, a2, g2, k_k, k_a, r_k, gn_g, gn_b)
    yb = spatial_gating(pu, pv, ln_g, ln_b, ws, bs)
    return jnp.concatenate([ya, yb], axis=-1) @ w_out


def rope(x):
    S = x.shape[1]
    half = x.shape[-1] // 2
    inv = ROPE_THETA ** (-jnp.arange(half, dtype=jnp.float32) / half)
    ang = jnp.arange(S, dtype=jnp.float32)[:, None] * inv[None, :]
    cos = jnp.cos(ang)[None, :, None, :]
    sin = jnp.sin(ang)[None, :, None, :]
    xf = x.astype(jnp.float32)
    x1, x2 = xf[..., :half], xf[..., half:]
    return jnp.concatenate([x1 * cos - x2 * sin, x1 * sin + x2 * cos], -1).astype(x.dtype)


def causal_block_attention(q, k, v):
    B, S, H, Dk = q.shape
    n_blk = S // Q_BLOCK
    scale = Dk ** -0.5
    qb = jnp.moveaxis(q.reshape(B, n_blk, Q_BLOCK, H, Dk), 1, 0)
    k_idx = jnp.arange(S)

    def one_block(args):
        q_blk, i = args
        s = jnp.einsum('bqhd,bkhd->bhqk', q_blk, k).astype(jnp.float32) * scale
        q_idx = i * Q_BLOCK + jnp.arange(Q_BLOCK)
        s = jnp.where(k_idx[None, :] <= q_idx[:, None], s, -jnp.inf)
        pr = jax.nn.softmax(s, axis=-1).astype(v.dtype)
        return jnp.einsum('bhqk,bkhd->bqhd', pr, v)

    o = lax.map(one_block, (qb, jnp.arange(n_blk)))
    return jnp.moveaxis(o, 0, 1).reshape(B, S, H, v.shape[-1])


def mla(x, w_in, q_norm, kv_norm, wq_b, wkv_b, w_out):
    B, S, _ = x.shape
    p = x @ w_in
    cq = p[..., :MLA_Q_RANK]
    ckv = p[..., MLA_Q_RANK:MLA_Q_RANK + MLA_KV_RANK]
    k_pe = p[..., MLA_Q_RANK + MLA_KV_RANK:]
    q = (rms_norm(cq, q_norm) @ wq_b).reshape(B, S, MLA_HEADS, MLA_QK)
    kv = (rms_norm(ckv, kv_norm) @ wkv_b).reshape(B, S, MLA_HEADS, MLA_NOPE + MLA_V)
    q_nope, q_pe = q[..., :MLA_NOPE], rope(q[..., MLA_NOPE:])
    k_nope, v = kv[..., :MLA_NOPE], kv[..., MLA_NOPE:]
    k_pe = rope(k_pe[:, :, None, :])
    qf = jnp.concatenate([q_nope, q_pe], -1)
    kf = jnp.concatenate([k_nope, jnp.broadcast_to(k_pe, (B, S, MLA_HEADS, MLA_ROPE))], -1)
    o = causal_block_attention(qf, kf, v)
    return o.reshape(B, S, MLA_MIX_DIM) @ w_out


def mem_cross_attention(x, mem, wq, wkv, wo):
    B, S, D = x.shape
    M = mem.shape[1]
    q = (x @ wq).reshape(B, S, XA_HEADS, XA_HEAD)
    kv = (mem @ wkv).reshape(B, M, 2, XA_HEADS, XA_HEAD)
    k, v = kv[:, :, 0], kv[:, :, 1]
    s = jnp.einsum('bqhd,bmhd->bhqm', q, k).astype(jnp.float32) * (XA_HEAD ** -0.5)
    pr = jax.nn.softmax(s, axis=-1).astype(v.dtype)
    o = jnp.einsum('bhqm,bmhd->bqhd', pr, v)
    return o.reshape(B, S, D) @ wo


def grouped_experts(xf, expert, gate, w_gate, w_up, w_down):
    T, D = xf.shape
    A = T * MOE_TOPK
    n_blk = -(-A // MOE_BLOCK) + MOE_EXPERTS
    L = n_blk * MOE_BLOCK
    flat_e = expert.reshape(-1)
    order = jnp.argsort(flat_e)
    sorted_e = flat_e[order]
    tok = (order // MOE_TOPK).astype(jnp.int32)
    counts = jnp.bincount(flat_e, length=MOE_EXPERTS)
    padded = (counts + MOE_BLOCK - 1) // MOE_BLOCK * MOE_BLOCK
    pad_end = jnp.cumsum(padded)
    pad_start = pad_end - padded
    start = jnp.cumsum(counts) - counts
    dest = (pad_start[sorted_e] + jnp.arange(A) - start[sorted_e]).astype(jnp.int32)
    row_tok = jnp.full((L,), T, dtype=jnp.int32).at[dest].set(tok)
    blk_expert = jnp.minimum(
        jnp.searchsorted(pad_end, jnp.arange(n_blk) * MOE_BLOCK, side='right'),
        MOE_EXPERTS - 1)
    x_rows = jnp.concatenate([xf, jnp.zeros((1, D), xf.dtype)], 0)[row_tok]
    x_rows = x_rows.reshape(n_blk, MOE_BLOCK, D)

    def expert_block(args):
        xb, e = args
        h = jax.nn.silu(xb @ w_gate[e]) * (xb @ w_up[e])
        return h @ w_down[e]

    y_rows = lax.map(expert_block, (x_rows, blk_expert)).reshape(L, D)
    y_assign = y_rows[dest] * gate.reshape(-1)[order][:, None]
    return jax.ops.segment_sum(y_assign, tok, num_segments=T)


def hier_moe(x, w_group, b_group, w_expert, b_expert, w_gate, w_up, w_down):
    B, S, D = x.shape
    xf = x.reshape(-1, D)
    T = xf.shape[0]
    g_logits = (xf @ w_group).astype(jnp.float32) + b_group
    g_prob = jax.nn.softmax(g_logits, axis=-1)
    grp = jnp.argmax(g_logits, axis=-1)
    p_grp = jnp.take_along_axis(g_prob, grp[:, None], axis=-1)
    e_logits = ((xf @ w_expert).astype(jnp.float32) + b_expert).reshape(
        T, MOE_GROUPS, MOE_PER_GROUP)
    e_logits = jnp.take_along_axis(e_logits, grp[:, None, None], axis=1)[:, 0]
    top_v, top_i = lax.top_k(e_logits, MOE_TOPK)
    gate = (jax.nn.softmax(top_v, axis=-1) * p_grp).astype(x.dtype)
    expert = (grp[:, None] * MOE_PER_GROUP + top_i).astype(jnp.int32)
    y = grouped_experts(xf, expert, gate, w_gate, w_up, w_down)
    return y.reshape(B, S, D)


def setup_inputs(seed: int = 0) -> dict:
    key = jax.random.key(seed)
    ks = iter(jax.random.split(key, 48))

    def normal(shape, scale):
        return jax.random.normal(next(ks), shape, jnp.float32) * scale

    def gain(shape):
        return 1.0 + normal(shape, 0.02)

    D = D_MODEL
    NE, NO, L = N_EVEN, N_ODD, DEPTH
    return {
        'x': normal((BATCH, SEQ, D), 1.0),
        'mem': normal((BATCH, MEM_LEN, D), 1.0),
        'ab_w_in': normal((NE, D, AB_IN_DIM), D ** -0.5),
        'ab_mu': jax.random.uniform(next(ks), (NE, AB_SHIFT_DIM), jnp.float32),
        'rw_w0': jax.random.uniform(next(ks), (NE, RW_DIM), jnp.float32, -6.5, -1.5),
        'rw_w2': normal((NE, RW_LORA_W, RW_DIM), 0.5 * RW_LORA_W ** -0.5),
        'rw_a0': normal((NE, RW_DIM), 0.3),
        'rw_a2': normal((NE, RW_LORA_A, RW_DIM), RW_LORA_A ** -0.5),
        'rw_g2': normal((NE, RW_LORA_G, RW_DIM), RW_LORA_G ** -0.5),
        'rw_k_k': 0.85 + normal((NE, RW_DIM), 0.05),
        'rw_k_a': 1.0 + normal((NE, RW_DIM), 0.05),
        'rw_r_k': normal((NE, RW_HEADS, RW_HEAD), 0.1),
        'rw_gn_g': gain((NE, RW_DIM)),
        'rw_gn_b': normal((NE, RW_DIM), 0.02),
        'sg_ln_g': gain((NE, SG_GROUPS, SG_GROUP_DIM)),
        'sg_ln_b': normal((NE, SG_GROUPS, SG_GROUP_DIM), 0.02),
        'sg_ws': normal((NE, SG_GROUPS, SG_CHUNK, SG_CHUNK), 0.5 * SG_CHUNK ** -0.5),
        'sg_b': 1.0 + normal((NE, SG_GROUPS, SG_CHUNK), 0.1),
        'ab_w_out': normal((NE, AB_MIX_DIM, D), DN_BETA * AB_MIX_DIM ** -0.5),
        'mla_w_in': normal((NO, D, MLA_IN_DIM), D ** -0.5),
        'mla_q_norm': gain((NO, MLA_Q_RANK)),
        'mla_kv_norm': gain((NO, MLA_KV_RANK)),
        'mla_wq_b': normal((NO, MLA_Q_RANK, MLA_HEADS * MLA_QK), MLA_Q_RANK ** -0.5),
        'mla_wkv_b': normal((NO, MLA_KV_RANK, MLA_HEADS * (MLA_NOPE + MLA_V)), MLA_KV_RANK ** -0.5),
        'mla_w_out': normal((NO, MLA_MIX_DIM, D), DN_BETA * MLA_MIX_DIM ** -0.5),
        'ln1_g': gain((L, D)),
        'ln1_b': normal((L, D), 0.02),
        'xa_wq': normal((L, D, D), D ** -0.5),
        'xa_wkv': normal((L, D, 2 * D), D ** -0.5),
        'xa_wo': normal((L, D, D), DN_BETA * D ** -0.5),
        'ln2_g': gain((L, D)),
        'ln2_b': normal((L, D), 0.02),
        'moe_w_group': normal((L, D, MOE_GROUPS), D ** -0.5),
        'moe_b_group': normal((L, MOE_GROUPS), 0.01),
        'moe_w_expert': normal((L, D, MOE_EXPERTS), D ** -0.5),
        'moe_b_expert': normal((L, MOE_EXPERTS), 0.01),
        'moe_w_gate': normal((L, MOE_EXPERTS, D, MOE_FF), D ** -0.5),
        'moe_w_up': normal((L, MOE_EXPERTS, D, MOE_FF), D ** -0.5),
        'moe_w_down': normal((L, MOE_EXPERTS, MOE_FF, D), DN_BETA * MOE_FF ** -0.5),
        'ln3_g': gain((L, D)),
        'ln3_b': normal((L, D), 0.02),
    }


def reference(x, mem, ab_w_in, ab_mu, rw_w0, rw_w2, rw_a0, rw_a2, rw_g2, rw_k_k,
              rw_k_a, rw_r_k, rw_gn_g, rw_gn_b, sg_ln_g, sg_ln_b, sg_ws, sg_b,
              ab_w_out, mla_w_in, mla_q_norm, mla_kv_norm, mla_wq_b, mla_wkv_b,
              mla_w_out, ln1_g, ln1_b, xa_wq, xa_wkv, xa_wo, ln2_g, ln2_b,
              moe_w_group, moe_b_group, moe_w_expert, moe_b_expert, moe_w_gate,
              moe_w_up, moe_w_down, ln3_g, ln3_b):
    h = x
    for layer in range(DEPTH):
        j = layer // 2
        if layer % 2 == 0:
            mix = mixer_ab(h, ab_w_in[j], ab_mu[j], rw_w0[j], rw_w2[j], rw_a0[j],
                           rw_a2[j], rw_g2[j], rw_k_k[j], rw_k_a[j], rw_r_k[j],
                           rw_gn_g[j], rw_gn_b[j], sg_ln_g[j], sg_ln_b[j], sg_ws[j],
                           sg_b[j], ab_w_out[j])
        else:
            mix = mla(h, mla_w_in[j], mla_q_norm[j], mla_kv_norm[j], mla_wq_b[j],
                      mla_wkv_b[j], mla_w_out[j])
        h = layer_norm(DN_ALPHA * h + mix, ln1_g[layer], ln1_b[layer])
        xa = mem_cross_attention(h, mem, xa_wq[layer], xa_wkv[layer], xa_wo[layer])
        h = layer_norm(DN_ALPHA * h + xa, ln2_g[layer], ln2_b[layer])
        ff = hier_moe(h, moe_w_group[layer], moe_b_group[layer], moe_w_expert[layer],
                      moe_b_expert[layer], moe_w_gate[layer], moe_w_up[layer],
                      moe_w_down[layer])
        h = layer_norm(DN_ALPHA * h + ff, ln3_g[layer], ln3_b[layer])
    return h
```

```python
import contextlib
import numpy as np
import concourse.bass as bass
import concourse.mybir as mybir
from concourse.bass_utils import run_bass_kernel_spmd

F32 = mybir.dt.float32
BF16 = mybir.dt.bfloat16
ALU = mybir.AluOpType
AF = mybir.ActivationFunctionType
AX = mybir.AxisListType

ENGS = ("pe", "act", "dve", "pool", "sp")
ALPHA = float(8 ** 0.25)
LN_EPS = 1e-5
NCORES = 8


class Buf:
    __slots__ = ("name", "w", "r")

    def __init__(self, name=""):
        self.name = name
        self.w = None
        self.r = {}


def bufs(n, name=""):
    return [Buf(f"{name}{i}") for i in range(n)]


class Sched:
    def __init__(self, nc, n_dma_sems=12):
        self.nc = nc
        self.q = {e: [] for e in ENGS}
        self.cnt = {e: 0 for e in ENGS}
        self.known = {e: {} for e in ENGS}
        self.semkeys = list(ENGS)
        self.ekey = {e: e for e in ENGS}
        self.epoch = 0
        self.ndma = n_dma_sems
        self.dma_val = {}
        self.dma_rr = {"sp": 0, "pool": 0, "act": 0}
        for qn in ("sp", "pool", "act"):
            for i in range(n_dma_sems):
                k = f"d_{qn}_{i}"
                self.semkeys.append(k)
                self.dma_val[k] = 0
        self.semkeys.append("cc")
        self.cc_val = 0
        self.n_ops = 0

    def _need(self, eng, reads, writes):
        need = {}

        def add(k, v):
            if need.get(k, 0) < v:
                need[k] = v

        for b in reads:
            if b.w is not None:
                add(b.w[0], b.w[1])
        for b in writes:
            if b.w is not None and not (b.w[2] == "pe" and eng == "pe"):
                add(b.w[0], b.w[1])
            for k, (v, e) in b.r.items():
                add(k, v)
        kn = self.known[eng]
        out = []
        for k, v in need.items():
            if kn.get(k, 0) < v:
                kn[k] = v
                out.append((k, v))
        return out

    def _commit(self, ev, reads, writes):
        k, v, e = ev
        for b in reads:
            if b.r.get(k, (0, e))[0] <= v:
                b.r[k] = (v, e)
        for b in writes:
            b.w = ev
            b.r = {}

    def op(self, eng, fn, reads=(), writes=(), sig=True):
        waits = self._need(eng, reads, writes)
        if sig:
            self.cnt[eng] += 1
            self.q[eng].append((waits, fn, (self.ekey[eng], 1)))
            v = self.cnt[eng]
        else:
            self.q[eng].append((waits, fn, None))
            v = self.cnt[eng] + 1
        self._commit((self.ekey[eng], v, eng), reads, writes)
        self.n_ops += 1

    def dma(self, queue, fn, reads=(), writes=()):
        i = self.dma_rr[queue]
        self.dma_rr[queue] = (i + 1) % self.ndma
        k = f"d_{queue}_{i}"
        waits = self._need(queue, reads, writes)
        prev = self.dma_val[k]
        kn = self.known[queue]
        if prev > 0 and kn.get(k, 0) < prev:
            kn[k] = prev
            waits.append((k, prev))
        self.dma_val[k] = prev + 16
        self.q[queue].append((waits, fn, (k, 16)))
        self._commit((k, prev + 16, "dma"), reads, writes)
        self.n_ops += 1

    def coll(self, fn, reads=(), writes=()):
        waits = self._need("pool", reads, writes)
        kn = self.known["pool"]
        if self.cc_val > 0 and kn.get("cc", 0) < self.cc_val:
            kn["cc"] = self.cc_val
            waits.append(("cc", self.cc_val))
        self.cc_val += 1
        self.q["pool"].append((waits, fn, ("cc", 1)))
        self._commit(("cc", self.cc_val, "dma"), reads, writes)
        self.n_ops += 1

    def barrier(self):
        cur = {self.ekey[e]: self.cnt[e] for e in ENGS}
        cur.update(self.dma_val)
        cur["cc"] = self.cc_val
        for eng in ENGS:
            kn = self.known[eng]
            waits = []
            for k, v in cur.items():
                if v > 0 and kn.get(k, 0) < v:
                    kn[k] = v
                    waits.append((k, v))
            if waits:
                self.q[eng].append((waits, None, None))

    def new_epoch(self):
        self.barrier()
        self.epoch += 1
        for e in ENGS:
            self.ekey[e] = f"{e}_{self.epoch}"
            self.semkeys.append(self.ekey[e])
            self.cnt[e] = 0

    def final_wait(self, eng, bufs_):
        waits = self._need(eng, bufs_, ())
        self.q[eng].append((waits, None, None))

    def run(self):
        nc = self.nc
        with contextlib.ExitStack() as st:
            sems = {}
            for k in self.semkeys:
                sems[k] = st.enter_context(nc.semaphore(k))
            block = st.enter_context(nc.Block())

            def replay(engname):
                def body(e):
                    for waits, fn, inc in self.q[engname]:
                        for (k, v) in waits:
                            e.wait_ge(sems[k], v)
                        if fn is not None:
                            ins = fn(e)
                            if inc is not None:
                                ins.then_inc(sems[inc[0]], inc[1])
                return body

            block.tensor(replay("pe"))
            block.scalar(replay("act"))
            block.vector(replay("dve"))
            block.gpsimd(replay("pool"))
            block.sync(replay("sp"))


class Region:
    def __init__(self, arena, start, size):
        self.arena, self.start, self.size, self.off = arena, start, size, 0

    def reset(self):
        self.off = 0

    def alloc(self, shape, dtype, parts=128):
        n = int(np.prod(shape))
        isz = 4 if dtype == F32 else 2
        nb = (n * isz + 63) // 64 * 64
        assert self.off + nb <= self.size, (self.off, nb, self.size)
        a = (self.start + self.off) // 4
        ap = self.arena[0:parts, a:a + nb // 4]
        self.off += nb
        if dtype != F32:
            ap = ap.bitcast(dtype)
        ap = ap[:, 0:n]
        if len(shape) == 2:
            ap = ap.rearrange("p (a b) -> p a b", a=shape[0])
        elif len(shape) == 3:
            ap = ap.rearrange("p (a b c) -> p a b c", a=shape[0], b=shape[1])
        return ap


class Ctx:
    pass


def make_ctx(nc, st, arena_bytes=206 * 1024):
    C = Ctx()
    C.nc = nc
    C.S = Sched(nc)
    C.arena = st.enter_context(nc.sbuf_tensor("arena", [128, arena_bytes // 4], F32))
    C.arena_bytes = arena_bytes
    C.ps = [st.enter_context(nc.psum_tensor(f"ps{i}", [128, 512], F32)) for i in range(8)]
    C.psB = bufs(8, "ps")
    C.ps_rr = 0
    return C


def next_ps(C):
    i = C.ps_rr
    C.ps_rr = (i + 1) % 8
    return C.ps[i], C.psB[i]


def MM(S, out, lhsT, rhs, start, stop, reads, writes):
    S.op("pe", lambda e: e.matmul(out, lhsT, rhs, start=start, stop=stop), reads, writes)


def TR(S, out, in_, ident, reads, writes):
    S.op("pe", lambda e: e.transpose(out, in_, ident), reads, writes)


def ACT(S, out, in_, func, reads, writes, bias=0.0, scale=1.0, accum_out=None):
    if accum_out is None:
        S.op("act", lambda e: e.activation(out=out, in_=in_, func=func, bias=bias, scale=scale), reads, writes)
    else:
        S.op("act", lambda e: e.activation(out=out, in_=in_, func=func, bias=bias, scale=scale,
                                           accum_out=accum_out), reads, writes)


def TT(S, eng, out, in0, in1, op, reads, writes):
    S.op(eng, lambda e: e.tensor_tensor(out, in0, in1, op), reads, writes)


def TS(S, eng, out, in0, s1, s2, op0, op1, reads, writes):
    if s2 is None:
        S.op(eng, lambda e: e.tensor_scalar(out, in0, s1, None, op0), reads, writes)
    else:
        S.op(eng, lambda e: e.tensor_scalar(out, in0, s1, s2, op0, op1), reads, writes)


def STT(S, eng, out, in0, scalar, in1, op0, op1, reads, writes):
    S.op(eng, lambda e: e.scalar_tensor_tensor(out, in0, scalar, in1, op0, op1), reads, writes)


def CP(S, eng, out, in_, reads, writes):
    if eng == "act":
        S.op("act", lambda e: e.copy(out, in_), reads, writes)
    else:
        S.op(eng, lambda e: e.tensor_copy(out, in_), reads, writes)


def DMA(S, q, out, in_, reads, writes):
    S.dma(q, lambda e: e.dma_start(out=out, in_=in_), reads, writes)


def load_w_bf16(C, reg, dram_ap, K, N, name, n0=0, n1=None):
    n1 = N if n1 is None else n1
    kc = K // 128
    t = reg.alloc([kc, n1 - n0], BF16)
    B = bufs(kc, name)
    src = dram_ap.rearrange("(c p) n -> p c n", p=128)
    for c in range(kc):
        DMA(C.S, "pool", t[:, c, :], src[:, c, n0:n1], [], [B[c]])
    return t, B


def ln_block(C, reg, reg2, z, zB, T, g_ap, b_ap, emit_out):
    S = C.S
    zb = reg.alloc([8, T], BF16)
    zs = reg.alloc([8, T], BF16)
    zbB, zsB = bufs(8, "zb"), bufs(8, "zs")
    for c in range(8):
        CP(S, "pool", zb[:, c, :], z[:, c, :], [zB[c]], [zbB[c]])
        ACT(S, zs[:, c, :], z[:, c, :], AF.Square, [zB[c]], [zsB[c]])
    p1, p1B = next_ps(C)
    p2, p2B = next_ps(C)
    for c in range(8):
        MM(S, p1[:, 0:T], C.ones_m[:, :], zb[:, c, :], c == 0, c == 7, [zbB[c], C.onesB], [p1B])
    for c in range(8):
        MM(S, p2[:, 0:T], C.ones_m[:, :], zs[:, c, :], c == 0, c == 7, [zsB[c], C.onesB], [p2B])
    mean = reg2.alloc([T], F32)
    rstd = reg2.alloc([T], F32)
    nmr = reg2.alloc([T], F32)
    mB, rB, nB = Buf("mean"), Buf("rstd"), Buf("nmr")
    CP(S, "act", mean, p1[:, 0:T], [p1B], [mB])
    STT(S, "dve", rstd, mean, -1.0, mean, ALU.mult, ALU.mult, [mB], [rB])
    TT(S, "dve", rstd, rstd, p2[:, 0:T], ALU.add, [rB, p2B], [rB])
    ACT(S, rstd, rstd, AF.Sqrt, [rB], [rB], bias=LN_EPS)
    S.op("dve", lambda e: e.reciprocal(rstd, rstd), [rB], [rB])
    STT(S, "dve", nmr, mean, -1.0, rstd, ALU.mult, ALU.mult, [mB, rB], [nB])
    t = reg2.alloc([2, T], F32)
    tB = bufs(2, "lnt")
    for c in range(8):
        k = c % 2
        TT(S, "dve", t[:, k, :], z[:, c, :], rstd, ALU.mult, [zB[c], rB], [tB[k]])
        TT(S, "pool", t[:, k, :], t[:, k, :], nmr, ALU.add, [tB[k], nB], [tB[k]])
        ACT(S, t[:, k, :], t[:, k, :], AF.Identity, [tB[k], C.lnpB], [tB[k]],
            bias=b_ap[:, c:c + 1], scale=g_ap[:, c:c + 1])
        emit_out(c, t[:, k, :], tB[k])


NT = 2048
TB = 512
NTB = NT // TB
NEXP = 32


def stage_P(C, D, n_exp=NEXP):
    S = C.S
    KB = 1024
    R_h = Region(C.arena, 0, 64 * KB)
    R_hb = Region(C.arena, 64 * KB, 32 * KB)
    R_y = Region(C.arena, 96 * KB, 64 * KB)
    R_t = Region(C.arena, 160 * KB, C.arena_bytes - 160 * KB)
    hres = R_h.alloc([8, NT], F32)
    hb = R_hb.alloc([8, NT], BF16)
    hresB = [bufs(NTB, f"hres{c}_") for c in range(8)]
    hbB = [bufs(NTB, f"hb{c}_") for c in range(8)]

    C.ones_m = R_t.alloc([128], BF16)
    C.onesB = Buf("ones")
    S.op("pool", lambda e: e.memset(C.ones_m, 1.0 / 1024.0), [], [C.onesB])
    ones1 = R_t.alloc([128], BF16)
    ones1B = Buf("ones1")
    S.op("pool", lambda e: e.memset(ones1, 1.0), [], [ones1B])
    lnp = R_t.alloc([6, 8], F32)
    C.lnpB = Buf("lnp")
    DMA(S, "sp", lnp, D["lnp"], [], [C.lnpB])
    ident = R_t.alloc([128], F32)
    identB = Buf("ident")
    DMA(S, "sp", ident, D["ident"], [], [identB])
    par = R_t.alloc([2], F32)
    parB = Buf("par")
    DMA(S, "sp", par, D["par"], [], [parB])
    kT = R_t.alloc([8, 256], BF16)
    vv = R_t.alloc([2, 1024], BF16)
    kTB, vvB = Buf("kT"), Buf("vv")
    t_mark = R_t.off

    for c in range(8):
        for tb in range(NTB):
            DMA(S, "sp", hres[:, c, tb * TB:(tb + 1) * TB], D["h_blk"](c, tb), D.get("h_deps", []), [hresB[c][tb]])

    memT, memB = load_w_bf16(C, R_y, D["memT"], 1024, 256, "memT")
    wk_, wkB_ = load_w_bf16(C, R_y, D["xa_wkv"], 1024, 2048, "wkvk", 0, 1024)
    wv_, wvB_ = load_w_bf16(C, R_y, D["xa_wkv"], 1024, 2048, "wkvv", 1024, 2048)
    for oc in range(8):
        ps, psB = next_ps(C)
        for kc in range(8):
            MM(S, ps[:, 0:256], wk_[:, kc, oc * 128:(oc + 1) * 128], memT[:, kc, :], kc == 0, kc == 7,
               [wkB_[kc], memB[kc]], [psB])
        CP(S, "act", kT[:, oc, :], ps[:, 0:256], [psB], [kTB])
    for mc in range(2):
        for nh in range(2):
            ps, psB = next_ps(C)
            for kc in range(8):
                MM(S, ps[:, :], memT[:, kc, mc * 128:(mc + 1) * 128], wv_[:, kc, nh * 512:(nh + 1) * 512],
                   kc == 0, kc == 7, [wvB_[kc], memB[kc]], [psB])
            CP(S, "act", vv[:, mc, nh * 512:(nh + 1) * 512], ps[:, :], [psB], [vvB])
    S.barrier()
    R_y.reset()

    wo_m, wo_mB = load_w_bf16(C, R_y, D["w_out"], 1024, 1024, "w_out")
    R_t.off = t_mark
    bl_a = R_t.alloc([8, TB], BF16)
    bl_b = R_t.alloc([8, TB], BF16)
    blB = [bufs(8, "bla"), bufs(8, "blb")]
    t_markA = R_t.off
    mxs = [R_y.alloc([8, TB], BF16) for _ in range(2)]
    mxB = [Buf("mx0"), Buf("mx1")]
    z = R_y.alloc([8, TB], F32)
    zB = bufs(8, "z")
    ln_mark = R_y.off
    for tb in range(NTB):
        tsl = slice(tb * TB, (tb + 1) * TB)
        mx, mB_ = mxs[tb % 2], mxB[tb % 2]
        for c in range(8):
            kind, fn, deps = D["mix_src"][c]
            if kind == "local":
                DMA(S, "sp", mx[:, c, :], fn(tb), deps, [mB_])
            else:
                DMA(S, "sp", bl_a[:, c, :], fn(tb, 0), deps, [blB[0][c]])
                DMA(S, "sp", bl_b[:, c, :], fn(tb, 1), deps, [blB[1][c]])
                TS(S, "dve", bl_a[:, c, :], bl_a[:, c, :], par[:, 0:1], None, ALU.mult, None, [blB[0][c], parB], [blB[0][c]])
                STT(S, "dve", mx[:, c, :], bl_b[:, c, :], par[:, 1:2], bl_a[:, c, :], ALU.mult, ALU.add,
                    [blB[1][c], blB[0][c], parB], [mB_])
        for oc in range(8):
            ps, psB = next_ps(C)
            for kc in range(8):
                MM(S, ps[:, :], wo_m[:, kc, oc * 128:(oc + 1) * 128], mx[:, kc, :], kc == 0, kc == 7,
                   [wo_mB[kc], mB_], [psB])
            STT(S, "dve", z[:, oc, :], hres[:, oc, tsl], ALPHA, ps[:, :], ALU.mult, ALU.add,
                [hresB[oc][tb], psB], [zB[oc]])
        R_y.off = ln_mark
        R_t.off = t_markA

        def out1(c, src, srcB, tb=tb, tsl=tsl):
            CP(S, "dve", hres[:, c, tsl], src, [srcB], [hresB[c][tb]])
            CP(S, "pool", hb[:, c, tsl], src, [srcB], [hbB[c][tb]])
        ln_block(C, R_y, R_t, z, zB, TB, lnp[:, 0, :], lnp[:, 1, :], out1)
    S.barrier()
    R_y.reset()

    wq, wqB = load_w_bf16(C, R_y, D["xa_wq"], 1024, 1024, "wq")
    wo, woB = load_w_bf16(C, R_y, D["xa_wo"], 1024, 1024, "wo")
    R_t.off = t_mark
    qT = R_t.alloc([8, TB], BF16)
    qTB = bufs(8, "qT")
    oT = R_t.alloc([8, TB], BF16)
    oTB = bufs(8, "oT")
    pT = R_t.alloc([4, TB], BF16)
    pTB = [bufs(2, "pTa"), bufs(2, "pTb")]
    rden = R_t.alloc([2, TB], F32)
    t_mark2 = R_t.off
    rdB = bufs(2, "rden")
    z = R_y.alloc([8, TB], F32)
    zB = bufs(8, "z2")
    ln_mark = R_y.off
    for tb in range(NTB):
        tsl = slice(tb * TB, (tb + 1) * TB)
        for oc in range(8):
            ps, psB = next_ps(C)
            for kc in range(8):
                MM(S, ps[:, :], wq[:, kc, oc * 128:(oc + 1) * 128], hb[:, kc, tsl], kc == 0, kc == 7,
                   [wqB[kc], hbB[kc][tb]], [psB])
            ACT(S, qT[:, oc, :], ps[:, :], AF.Copy, [psB], [qTB[oc]], scale=1.0 / 16.0)
        for hh in range(4):
            pb = hh % 2
            for mc in range(2):
                ps, psB = next_ps(C)
                for j in range(2):
                    MM(S, ps[:, :], kT[:, 2 * hh + j, mc * 128:(mc + 1) * 128], qT[:, 2 * hh + j, :],
                       j == 0, j == 1, [kTB, qTB[2 * hh + j]], [psB])
                ACT(S, pT[:, pb * 2 + mc, :], ps[:, :], AF.Exp, [psB], [pTB[pb][mc]])
            dps, dpsB = next_ps(C)
            for mc in range(2):
                MM(S, dps[:, :], ones1[:, :], pT[:, pb * 2 + mc, :], mc == 0, mc == 1,
                   [ones1B, pTB[pb][mc]], [dpsB])
            S.op("dve", (lambda o, i: (lambda e: e.reciprocal(o, i)))(rden[:, pb, :], dps[:, :]), [dpsB], [rdB[pb]])
            for j in range(2):
                ps, psB = next_ps(C)
                for mc in range(2):
                    MM(S, ps[:, :], vv[:, mc, hh * 256 + j * 128: hh * 256 + (j + 1) * 128], pT[:, pb * 2 + mc, :],
                       mc == 0, mc == 1, [vvB, pTB[pb][mc]], [psB])
                TT(S, "dve", oT[:, 2 * hh + j, :], ps[:, :], rden[:, pb, :], ALU.mult, [psB, rdB[pb]], [oTB[2 * hh + j]])
        for oc in range(8):
            ps, psB = next_ps(C)
            for kc in range(8):
                MM(S, ps[:, :], wo[:, kc, oc * 128:(oc + 1) * 128], oT[:, kc, :], kc == 0, kc == 7,
                   [woB[kc], oTB[kc]], [psB])
            STT(S, "dve", z[:, oc, :], hres[:, oc, tsl], ALPHA, ps[:, :], ALU.mult, ALU.add,
                [hresB[oc][tb], psB], [zB[oc]])
        R_y.off = ln_mark
        R_t.off = t_mark2

        def out2(c, src, srcB, tb=tb, tsl=tsl):
            CP(S, "dve", hres[:, c, tsl], src, [srcB], [hresB[c][tb]])
            CP(S, "pool", hb[:, c, tsl], src, [srcB], [hbB[c][tb]])
        ln_block(C, R_y, R_t, z, zB, TB, lnp[:, 2, :], lnp[:, 3, :], out2)
    S.barrier()
    R_y.reset()

    BIG = 1.0e4
    R_t.off = t_mark
    wr = R_t.alloc([8, 36], F32)
    wrB = Buf("wr")
    DMA(S, "sp", wr, D["w_r"].rearrange("(c p) n -> p c n", p=128), [], [wrB])
    brb = R_t.alloc([36], F32)
    brB = Buf("br")
    DMA(S, "sp", brb, D["b_r"].partition_broadcast(128), [], [brB])
    sel = R_t.alloc([32, 128], F32, parts=32)
    selB = Buf("sel")
    DMA(S, "sp", sel, D["sel"].rearrange("k (e p) -> k e p", p=128), [], [selB])
    GT = R_t.alloc([NT], F32, parts=32)
    GTB = bufs(NTB, "GT")
    NTT = NT // 128
    rt = R_y
    L = rt.alloc([NTT, 36], F32)
    Em = rt.alloc([NTT, 32], F32)
    oh1 = rt.alloc([NTT, 32], F32)
    oh2 = rt.alloc([NTT, 32], F32)
    G = rt.alloc([NTT, 32], F32)
    sm = rt.alloc([NTT, 16], F32)
    tB_ = [Buf(f"rt{i}") for i in range(NTT)]
    for tt in range(NTT):
        tb = tt // 4
        B_ = tB_[tt]
        ps, psB = next_ps(C)
        for kc in range(8):
            MM(S, ps[:, 0:36], hres[:, kc, tt * 128:(tt + 1) * 128], wr[:, kc, :], kc == 0, kc == 7,
               [hresB[kc][tb], wrB], [psB])
        Lt, Et, o1, o2, Gt, s = L[:, tt, :], Em[:, tt, :], oh1[:, tt, :], oh2[:, tt, :], G[:, tt, :], sm[:, tt, :]
        TT(S, "dve", Lt, ps[:, 0:36], brb, ALU.add, [psB, brB], [B_])
        S.op("dve", (lambda o, i: (lambda e: e.reduce_max(o, i, AX.X)))(s[:, 0:1], Lt[:, 0:4]), [B_], [B_])
        TS(S, "dve", s[:, 4:8], Lt[:, 0:4], s[:, 0:1], None, ALU.is_equal, ALU.bypass, [B_], [B_])
        TS(S, "dve", s[:, 4:8], s[:, 4:8], BIG, -BIG, ALU.mult, ALU.add, [B_], [B_])
        TS(S, "dve", s[:, 1:2], s[:, 0:1], -1.0, None, ALU.mult, ALU.bypass, [B_], [B_])
        ACT(S, s[:, 8:12], Lt[:, 0:4], AF.Exp, [B_], [B_], bias=s[:, 1:2], accum_out=s[:, 2:3])
        S.op("dve", (lambda o, i: (lambda e: e.reciprocal(o, i)))(s[:, 3:4], s[:, 2:3]), [B_], [B_])
        for g in range(4):
            TS(S, "dve", Et[:, g * 8:(g + 1) * 8], Lt[:, 4 + g * 8: 12 + g * 8], s[:, 4 + g:5 + g], None,
               ALU.add, ALU.bypass, [B_], [B_])
        S.op("dve", (lambda o, i: (lambda e: e.reduce_max(o, i, AX.X)))(s[:, 12:13], Et), [B_], [B_])
        TS(S, "dve", o1, Et, s[:, 12:13], None, ALU.is_equal, ALU.bypass, [B_], [B_])
        STT(S, "dve", Et, o1, -BIG, Et, ALU.mult, ALU.add, [B_], [B_])
        S.op("dve", (lambda o, i: (lambda e: e.reduce_max(o, i, AX.X)))(s[:, 13:14], Et), [B_], [B_])
        TS(S, "dve", o2, Et, s[:, 13:14], None, ALU.is_equal, ALU.bypass, [B_], [B_])
        TS(S, "dve", s[:, 14:15], s[:, 12:13], -1.0, None, ALU.mult, ALU.bypass, [B_], [B_])
        ACT(S, s[:, 15:16], s[:, 13:14], AF.Exp, [B_], [B_], bias=s[:, 14:15])
        TS(S, "dve", s[:, 14:15], s[:, 15:16], 1.0, None, ALU.add, ALU.bypass, [B_], [B_])
        S.op("dve", (lambda o, i: (lambda e: e.reciprocal(o, i)))(s[:, 14:15], s[:, 14:15]), [B_], [B_])
        TT(S, "dve", s[:, 14:15], s[:, 14:15], s[:, 3:4], ALU.mult, [B_], [B_])
        TT(S, "dve", s[:, 15:16], s[:, 15:16], s[:, 14:15], ALU.mult, [B_], [B_])
        TS(S, "dve", Gt, o1, s[:, 14:15], None, ALU.mult, ALU.bypass, [B_], [B_])
        STT(S, "dve", Gt, o2, s[:, 15:16], Gt, ALU.mult, ALU.add, [B_], [B_])
        tp, tpB = next_ps(C)
        TR(S, tp[0:32, 0:128], Gt, ident[:, :], [B_, identB], [tpB])
        CP(S, "act", GT[:, tt * 128:(tt + 1) * 128], tp[0:32, 0:128], [tpB], [GTB[tb]])
    S.barrier()
    R_y.reset()

    yacc = R_y.alloc([8, NT], F32)
    yB = [bufs(NTB, f"y{c}_") for c in range(8)]
    for c in range(8):
        for tb in range(NTB):
            tsl = slice(tb * TB, (tb + 1) * TB)
            ACT(S, yacc[:, c, tsl], hres[:, c, tsl], AF.Copy, [hresB[c][tb]], [yB[c][tb]], scale=ALPHA)
    S.barrier()
    R_h.reset()
    wbuf = []
    for i in range(2):
        wg = R_h.alloc([8, 512], BF16)
        wu = R_h.alloc([8, 512], BF16)
        wd = R_h.alloc([4, 1024], BF16)
        wbuf.append((wg, wu, wd, bufs(8, f"wg{i}"), bufs(8, f"wu{i}"), bufs(4, f"wd{i}")))
    sg = [R_h.alloc([TB], F32) for _ in range(2)]
    sgB = bufs(2, "sg")
    tt_ = [R_h.alloc([TB], F32) for _ in range(2)]
    ttB = bufs(2, "tt")
    gbs = [R_h.alloc([TB], F32) for _ in range(2)]
    gbB = bufs(2, "gb")
    hg = [R_t.alloc([4, TB], BF16) for _ in range(2)]
    hgB = [bufs(4, "hga"), bufs(4, "hgb")]
    P_G, P_U, P_D, P_B = (0, 1), (2, 3), (4, 5), (6, 7)
    wgs = D["moe_wg"]
    wus = D["moe_wu"]
    wds = D["moe_wd"]

    def load_expert(e):
        wg, wu, wd, bg, bu, bd = wbuf[e % 2]
        sg_ = wgs[e].rearrange("(c p) n -> p c n", p=128)
        su_ = wus[e].rearrange("(c p) n -> p c n", p=128)
        sd_ = wds[e].rearrange("(c p) n -> p c n", p=128)
        for c in range(0, 8, 2):
            DMA(S, "pool", wg[:, c:c + 2, :], sg_[:, c:c + 2, :], [], [bg[c], bg[c + 1]])
        for c in range(0, 8, 2):
            DMA(S, "pool", wu[:, c:c + 2, :], su_[:, c:c + 2, :], [], [bu[c], bu[c + 1]])
        for c in range(4):
            DMA(S, "pool", wd[:, c, :], sd_[:, c, :], [], [bd[c]])

    def down_part(e, tb, it):
        wg, wu, wd, bg, bu, bd = wbuf[e % 2]
        tsl = slice(tb * TB, (tb + 1) * TB)
        hgi, hgBi = hg[it % 2], hgB[it % 2]
        for dc in range(8):
            pi = P_D[dc % 2]
            ps, psB = C.ps[pi], C.psB[pi]
            for f in range(4):
                MM(S, ps[:, :], wd[:, f, dc * 128:(dc + 1) * 128], hgi[:, f, :], f == 0, f == 3,
                   [bd[f], hgBi[f]], [psB])
            TT(S, "dve", yacc[:, dc, tsl], yacc[:, dc, tsl], ps[:, :], ALU.add, [yB[dc][tb], psB], [yB[dc][tb]])

    load_expert(0)
    it = 0
    prev = None
    for e in range(n_exp):
        wg, wu, wd, bg, bu, bd = wbuf[e % 2]
        for tb in range(NTB):
            tsl = slice(tb * TB, (tb + 1) * TB)
            k2 = it % 2
            pbi = P_B[k2]
            MM(S, C.ps[pbi][:, :], sel[:, e, :], GT[:, tsl], True, True, [selB, GTB[tb]], [C.psB[pbi]])
            CP(S, "act", gbs[k2], C.ps[pbi][:, :], [C.psB[pbi]], [gbB[k2]])
            hgi, hgBi = hg[k2], hgB[k2]
            for f in range(4):
                pg, pu = P_G[f % 2], P_U[f % 2]
                for kc in range(8):
                    MM(S, C.ps[pg][:, :], wg[:, kc, f * 128:(f + 1) * 128], hb[:, kc, tsl], kc == 0, kc == 7,
                       [bg[kc], hbB[kc][tb]], [C.psB[pg]])
                for kc in range(8):
                    MM(S, C.ps[pu][:, :], wu[:, kc, f * 128:(f + 1) * 128], hb[:, kc, tsl], kc == 0, kc == 7,
                       [bu[kc], hbB[kc][tb]], [C.psB[pu]])
                f2 = f % 2
                ACT(S, sg[f2], C.ps[pg][:, :], AF.Silu, [C.psB[pg]], [sgB[f2]])
                TT(S, "dve", tt_[f2], C.ps[pu][:, :], gbs[k2], ALU.mult, [C.psB[pu], gbB[k2]], [ttB[f2]])
                TT(S, "dve", hgi[:, f, :], tt_[f2], sg[f2], ALU.mult, [ttB[f2], sgB[f2]], [hgBi[f]])
            if prev is not None:
                down_part(*prev)
            if tb == 0 and e + 1 < n_exp:
                load_expert(e + 1)
            prev = (e, tb, it)
            it += 1
    down_part(*prev)
    S.barrier()
    R_h.reset()

    outBs = []
    stg16 = [R_h.alloc([TB], BF16) for _ in range(4)]
    stg16B = bufs(4, "stg16")
    stg = [R_h.alloc([TB], F32) for _ in range(4)]
    stgB = bufs(4, "stg")
    cnt = [0]
    ln_mark = R_h.off
    for tb in range(NTB):
        tsl = slice(tb * TB, (tb + 1) * TB)
        R_h.off = ln_mark

        def out3(c, src, srcB, tb=tb, tsl=tsl):
            i = cnt[0] % 4
            cnt[0] += 1
            CP(S, "dve", stg[i], src, [srcB], [stgB[i]])
            ob = Buf("out")
            outBs.append(ob)
            DMA(S, "sp", D["out_blk"](c, tb), stg[i], [stgB[i]], [ob])
            if "out16_blk" in D:
                CP(S, "pool", stg16[i], src, [srcB], [stg16B[i]])
                ob = Buf("out16")
                outBs.append(ob)
                DMA(S, "sp", D["out16_blk"](c, tb), stg16[i], [stg16B[i]], [ob])
        n_before = len(outBs)
        ln_block(C, R_h, R_h, yacc[:, :, tsl], [yB[c][tb] for c in range(8)], TB, lnp[:, 4, :], lnp[:, 5, :], out3)
        if "after_tb" in D:
            D["after_tb"](tb, outBs[n_before:])
    return outBs


def consts_P():
    sel = np.zeros((32, 32, 128), np.float32)
    for e in range(32):
        sel[e, e, :] = 1.0
    return {"sel": sel.reshape(32, 32 * 128), "ident": np.eye(128, dtype=np.float32)}


def lnp_layout(vs):
    return np.ascontiguousarray(np.stack([v.reshape(8, 128).T for v in vs], axis=1)).astype(np.float32)


SEQ = 4096
NQB = SEQ // 512
RMS_EPS = 1e-6
HPC = 8


def stage_O(C, D):
    S = C.S
    KB = 1024
    R1 = Region(C.arena, 0, 64 * KB)
    R2 = Region(C.arena, 64 * KB, 100 * KB)
    R3 = Region(C.arena, 164 * KB, C.arena_bytes - 164 * KB)
    cqn = R2.alloc([2, SEQ], BF16)
    ckvn = R2.alloc([2, SEQ], BF16)
    kpeT = R2.alloc([SEQ], BF16)
    cosT = R2.alloc([SEQ], F32)
    sinT = R2.alloc([SEQ], F32)
    cqnB = [bufs(NQB, "cqn0_"), bufs(NQB, "cqn1_")]
    ckvB = [bufs(NQB, "ckv0_"), bufs(NQB, "ckv1_")]
    kpeB = bufs(NQB, "kpe")
    tabB = Buf("tab")
    DMA(S, "sp", cosT[0:96, :], D["cosT"], [], [tabB])
    DMA(S, "sp", sinT[0:96, :], D["sinT"], [], [tabB])
    nrm = R2.alloc([4], F32)
    nrmB = Buf("nrm")
    DMA(S, "sp", nrm, D["nrm"], [], [nrmB])
    onesm = R2.alloc([128], BF16)
    onesB = Buf("ones")
    S.op("pool", lambda e: e.memset(onesm, 1.0 / 256.0), [], [onesB])
    masks = R2.alloc([4, 512], BF16)
    maskB = Buf("mask")
    DMA(S, "pool", masks, D["masks"].rearrange("p (a b) -> p a b", a=4), [], [maskB])

    hb = R1.alloc([8, SEQ], BF16)
    hbB = [bufs(NQB, f"hb{c}_") for c in range(8)]
    for tb in range(NQB):
        for c in range(8):
            DMA(S, "sp", hb[:, c, tb * 512:(tb + 1) * 512], D["h_blk"](c, tb), D["h_deps"](tb), [hbB[c][tb]])
    win, winB = load_w_bf16(C, R3, D["w_in"], 1024, 704, "win")
    cf = R3.alloc([4, 512], F32)
    cfB = bufs(4, "cf")
    sq = R3.alloc([4, 512], BF16)
    sqB = bufs(4, "sq")
    rs = R3.alloc([2, 512], F32)
    rsB = bufs(2, "rs")
    rt = R3.alloc([2, 512], F32)
    rtB = bufs(2, "rt")
    for tb in range(NQB):
        tsl = slice(tb * 512, (tb + 1) * 512)
        for oc in range(4):
            ps, psB = next_ps(C)
            for kc in range(8):
                MM(S, ps[:, :], win[:, kc, oc * 128:(oc + 1) * 128], hb[:, kc, tsl], kc == 0, kc == 7,
                   [winB[kc], hbB[kc][tb]], [psB])
            CP(S, "act", cf[:, oc, :], ps[:, :], [psB], [cfB[oc]])
            TT(S, "pool", sq[:, oc, :], cf[:, oc, :], cf[:, oc, :], ALU.mult, [cfB[oc]], [sqB[oc]])
        for lat in range(2):
            ps, psB = next_ps(C)
            for j in range(2):
                MM(S, ps[:, :], onesm[:, :], sq[:, 2 * lat + j, :], j == 0, j == 1, [onesB, sqB[2 * lat + j]], [psB])
            ACT(S, rs[:, lat, :], ps[:, :], AF.Sqrt, [psB], [rsB[lat]], bias=RMS_EPS)
            S.op("dve", (lambda o: (lambda e: e.reciprocal(o, o)))(rs[:, lat, :]), [rsB[lat]], [rsB[lat]])
            for j in range(2):
                oc = 2 * lat + j
                TT(S, "dve", cf[:, oc, :], cf[:, oc, :], rs[:, lat, :], ALU.mult, [cfB[oc], rsB[lat]], [cfB[oc]])
                dst, dB = (cqn, cqnB) if lat == 0 else (ckvn, ckvB)
                ACT(S, dst[:, j, tsl], cf[:, oc, :], AF.Identity, [cfB[oc], nrmB], [dB[j][tb]],
                    scale=nrm[:, oc:oc + 1])
        ps, psB = next_ps(C)
        ps2, ps2B = next_ps(C)
        for kc in range(8):
            MM(S, ps[0:96, :], win[:, kc, 512:608], hb[:, kc, tsl], kc == 0, kc == 7, [winB[kc], hbB[kc][tb]], [psB])
        for kc in range(8):
            MM(S, ps2[0:96, :], win[:, kc, 608:704], hb[:, kc, tsl], kc == 0, kc == 7, [winB[kc], hbB[kc][tb]], [ps2B])
        TT(S, "dve", rt[64:96, 0, :], ps[64:96, :], cosT[64:96, tsl], ALU.mult, [psB, tabB], [rtB[0]])
        TT(S, "dve", rt[64:96, 1, :], ps2[64:96, :], sinT[64:96, tsl], ALU.mult, [ps2B, tabB], [rtB[1]])
        TT(S, "pool", kpeT[64:96, tsl], rt[64:96, 0, :], rt[64:96, 1, :], ALU.add, [rtB[0], rtB[1]], [kpeB[tb]])
    S.barrier()
    R1.reset()
    R3.reset()

    wq, wqB = load_w_bf16(C, R3, D["wq"], 256, HPC * 96, "wq")
    wqs, wqsB = load_w_bf16(C, R3, D["wq_sw"], 256, HPC * 96, "wqs")
    wkk, wkkB = load_w_bf16(C, R3, D["wkv_k"], 256, HPC * 64, "wkk")
    wkv, wkvB = load_w_bf16(C, R3, D["wkv_v"], 256, HPC * 64, "wkv")
    kT = [R1.alloc([SEQ], BF16) for _ in range(2)]
    qT = [R1.alloc([SEQ], BF16) for _ in range(2)]
    V = [R1.alloc([32, 128], BF16) for _ in range(2)]
    kTB = [bufs(NQB, "kTa"), bufs(NQB, "kTb")]
    kT2B = bufs(2, "kTpe")
    qTB = [bufs(NQB, "qTa"), bufs(NQB, "qTb")]
    VB = [bufs(4, "Va"), bufs(4, "Vb")]
    V1B = bufs(2, "Vones")
    for i in range(2):
        S.op("pool", (lambda ap: (lambda e: e.memset(ap, 1.0)))(V[i][:, :, 64:128]), [], [V1B[i]])
    pT = [R3.alloc([512], BF16) for _ in range(4)]
    pTB = bufs(4, "pT")
    qr = R3.alloc([2, 512], F32)
    qrB = bufs(2, "qr")
    rden = [R3.alloc([512], F32) for _ in range(2)]
    rdB = bufs(2, "rden")
    ost = [R3.alloc([512], BF16) for _ in range(2)]
    ostB = bufs(2, "ost")
    outBs = []
    scale = float(96 ** -0.5)
    PS_S = (0, 1, 2, 5)
    PS_O = (3, 4)
    PS_X = (6, 7)
    xi = [0]

    def xps():
        i = PS_X[xi[0] % 2]
        xi[0] += 1
        return C.ps[i], C.psB[i]

    def prep_head(h):
        b = h % 2
        for tb in range(NQB):
            tsl = slice(tb * 512, (tb + 1) * 512)
            ps, psB = xps()
            for kc in range(2):
                MM(S, ps[0:64, :], wkk[:, kc, h * 64:(h + 1) * 64], ckvn[:, kc, tsl], kc == 0, kc == 1,
                   [wkkB[kc], ckvB[kc][tb]], [psB])
            CP(S, "act", kT[b][0:64, tsl], ps[0:64, :], [psB], [kTB[b][tb]])
        CP(S, "pool", kT[b][64:96, :], kpeT[64:96, :], kpeB, [kT2B[b]])
        for g in range(4):
            ps, psB = xps()
            for j in range(8):
                kt = g * 8 + j
                for kc in range(2):
                    MM(S, ps[:, j * 64:(j + 1) * 64], ckvn[:, kc, kt * 128:(kt + 1) * 128],
                       wkv[:, kc, h * 64:(h + 1) * 64], kc == 0, kc == 1,
                       [wkvB[kc], ckvB[kc][kt // 4]], [psB])
            CP(S, "dve", V[b][:, g * 8:(g + 1) * 8, 0:64], ps[:, :].rearrange("p (a b) -> p a b", a=8),
               [psB], [VB[b][g]])
        for tb in range(NQB):
            tsl = slice(tb * 512, (tb + 1) * 512)
            ps, psB = xps()
            ps2, ps2B = xps()
            for kc in range(2):
                MM(S, ps[0:96, :], wq[:, kc, h * 96:(h + 1) * 96], cqn[:, kc, tsl], kc == 0, kc == 1,
                   [wqB[kc], cqnB[kc][tb]], [psB])
            for kc in range(2):
                MM(S, ps2[0:96, :], wqs[:, kc, h * 96:(h + 1) * 96], cqn[:, kc, tsl], kc == 0, kc == 1,
                   [wqsB[kc], cqnB[kc][tb]], [ps2B])
            TS(S, "dve", qT[b][0:64, tsl], ps[0:64, :], scale, None, ALU.mult, None, [psB], [qTB[b][tb]])
            STT(S, "dve", qr[64:96, 0, :], ps[64:96, :], scale, cosT[64:96, tsl], ALU.mult, ALU.mult,
                [psB, tabB], [qrB[0]])
            STT(S, "dve", qr[64:96, 1, :], ps2[64:96, :], scale, sinT[64:96, tsl], ALU.mult, ALU.mult,
                [ps2B, tabB], [qrB[1]])
            TT(S, "pool", qT[b][64:96, tsl], qr[64:96, 0, :], qr[64:96, 1, :], ALU.add, [qrB[0], qrB[1]],
               [qTB[b][tb]])

    DEPTH = 3
    for h in range(HPC):
        prep_head(h)
        b = h % 2
        pairs = [(qb, kt) for qb in range(NQB) for kt in range(4 * qb + 4)]

        def emit_qk(i, b=b):
            qb, kt = pairs[i]
            i4 = i % 4
            qsl = slice(qb * 512, (qb + 1) * 512)
            s_ps, s_psB = C.ps[PS_S[i4]], C.psB[PS_S[i4]]
            MM(S, s_ps[:, :], kT[b][0:96, kt * 128:(kt + 1) * 128], qT[b][0:96, qsl], True, True,
               [kTB[b][kt // 4], kT2B[b], qTB[b][qb]], [s_psB])
            ACT(S, pT[i4], s_ps[:, :], AF.Exp, [s_psB], [pTB[i4]])
            if kt >= 4 * qb:
                TT(S, "pool", pT[i4], pT[i4], masks[:, kt - 4 * qb, :], ALU.mult, [pTB[i4], maskB], [pTB[i4]])

        def emit_pv(i, b=b, h=h):
            qb, kt = pairs[i]
            i4 = i % 4
            qsl = slice(qb * 512, (qb + 1) * 512)
            nkt = 4 * qb + 4
            po = PS_O[qb % 2]
            o_ps, o_psB = C.ps[po], C.psB[po]
            MM(S, o_ps[:, :], V[b][:, kt, :], pT[i4], kt == 0, kt == nkt - 1,
               [VB[b][kt // 8], V1B[b], pTB[i4]], [o_psB])
            if kt == nkt - 1:
                r2 = qb % 2
                S.op("dve", (lambda o, i_: (lambda e: e.reciprocal(o, i_)))(rden[r2][64:128, :], o_ps[64:128, :]),
                     [o_psB], [rdB[r2]])
                TT(S, "dve", ost[r2][0:64, :], o_ps[0:64, :], rden[r2][64:128, :], ALU.mult, [o_psB, rdB[r2]], [ostB[r2]])
                ob = Buf("out")
                outBs.append(ob)
                DMA(S, "sp", D["o_blk"](h, qb), ost[r2][0:64, :], [ostB[r2]], [ob])

        n_before = len(outBs)
        for i in range(len(pairs) + DEPTH):
            if i < len(pairs):
                emit_qk(i)
            if i >= DEPTH:
                emit_pv(i - DEPTH)
        if "after_head" in D:
            D["after_head"](h, outBs[n_before:])
    return outBs


def consts_O():
    half = 16
    inv = (10000.0 ** (-np.arange(half, dtype=np.float32) / half)).astype(np.float32)
    ang = np.arange(SEQ, dtype=np.float32)[:, None] * inv[None, :]
    cos, sin = np.cos(ang).astype(np.float32), np.sin(ang).astype(np.float32)
    cosT = np.zeros((96, SEQ), np.float32)
    sinT = np.zeros((96, SEQ), np.float32)
    cosT[64:80] = cos.T; cosT[80:96] = cos.T
    sinT[64:80] = -sin.T; sinT[80:96] = sin.T
    p = np.arange(128)[:, None]
    f = np.arange(512)[None, :]
    masks = np.concatenate([(f >= p + 128 * d).astype(np.float32) for d in range(4)], axis=1)
    return {"cosT": cosT, "sinT": sinT, "masks": np.ascontiguousarray(masks)}


def prep_O_weights(w_in, q_norm, kv_norm, wq_b, wkv_b, hs):
    pad = np.zeros((1024, 64), np.float32)
    kpe = w_in[:, 512:544]
    kpe_sw = np.concatenate([kpe[:, 16:32], kpe[:, 0:16]], axis=1)
    w_in_ext = np.ascontiguousarray(np.concatenate([w_in[:, 0:512], pad, kpe, pad, kpe_sw], axis=1))
    nrm = np.ascontiguousarray(np.concatenate([q_norm.reshape(2, 128).T, kv_norm.reshape(2, 128).T], axis=1))
    heads = range(hs * HPC, (hs + 1) * HPC)
    wq = np.concatenate([wq_b[:, h * 96:(h + 1) * 96] for h in heads], axis=1)
    wq_sw = np.concatenate([np.concatenate([wq_b[:, h * 96:h * 96 + 64], wq_b[:, h * 96 + 80:h * 96 + 96],
                                            wq_b[:, h * 96 + 64:h * 96 + 80]], axis=1) for h in heads], axis=1)
    wkv_k = np.concatenate([wkv_b[:, h * 128:h * 128 + 64] for h in heads], axis=1)
    wkv_v = np.concatenate([wkv_b[:, h * 128 + 64:(h + 1) * 128] for h in heads], axis=1)
    return dict(w_in=w_in_ext, nrm=nrm.astype(np.float32), wq=np.ascontiguousarray(wq), wq_sw=np.ascontiguousarray(wq_sw),
                wkv_k=np.ascontiguousarray(wkv_k), wkv_v=np.ascontiguousarray(wkv_v))


TBR = 256
NBR = SEQ // TBR
CH = 128
GELU_C = 1.5957691216057308
NEG_EHALF = -0.6065306597126334
GN_EPS = 64e-5


def gelu_ops(S, dst, x, tmp1, tmp2, xB, t1B, t2B, dstB):
    TT(S, "pool", tmp1, x, x, ALU.mult, [xB], [t1B])
    TS(S, "dve", tmp1, tmp1, 0.044715, 1.0, ALU.mult, ALU.add, [t1B], [t1B])
    TT(S, "pool", tmp1, tmp1, x, ALU.mult, [t1B, xB], [t1B])
    ACT(S, tmp2, tmp1, AF.Sigmoid, [t1B], [t2B], scale=GELU_C)
    TT(S, "dve", dst, x, tmp2, ALU.mult, [xB, t2B], [dstB])


def stage_E(C, D):
    S = C.S
    KB = 1024
    R1 = Region(C.arena, 0, 64 * KB)
    R2 = Region(C.arena, 64 * KB, C.arena_bytes - 64 * KB)
    hb = R1.alloc([8, SEQ], BF16)
    hbB = [bufs(SEQ // 512, f"hb{c}_") for c in range(8)]
    for tb in range(SEQ // 512):
        for c in range(8):
            DMA(S, "sp", hb[:, c, tb * 512:(tb + 1) * 512], D["h_blk"](c, tb), D["h_deps"](tb), [hbB[c][tb]])
    ident = R2.alloc([4, 128], F32)
    cB = Buf("consts")
    for i in range(4):
        DMA(S, "sp", ident[:, i, :], D["ident"], [], [cB])
    mLQ = R2.alloc([2, 256], F32)
    for i in range(2):
        DMA(S, "sp", mLQ[:, i, :], D["maskLQ"], [], [cB])
    mSL = R2.alloc([4, 128], F32)
    for i in range(4):
        DMA(S, "sp", mSL[:, i, :], D["maskSL"], [], [cB])
    blk = R2.alloc([128], F32)
    DMA(S, "sp", blk, D["blk"], [], [cB])
    blk64 = R2.alloc([128], F32)
    S.op("dve", lambda e: e.tensor_scalar(blk64, blk, 1.0 / 64.0, None, ALU.mult), [cB], [cB])
    ones = R2.alloc([128], F32)
    S.op("pool", lambda e: e.memset(ones, 1.0), [], [cB])
    c_mark = R2.off

    hsg = R2.alloc([8, 2048], BF16)
    hsgB = [bufs(4, f"hsg{c}_") for c in range(8)]
    for tb in range(4):
        for c in range(8):
            DMA(S, "sp", hsg[:, c, tb * 512:(tb + 1) * 512], D["hsg_blk"](c, tb), [], [hsgB[c][tb]])
    wpu, wpuB = load_w_bf16(C, R2, D["w_pu"], 1024, 512, "wpu")
    wpv, wpvB = load_w_bf16(C, R2, D["w_pv"], 1024, 512, "wpv")
    wsf = R2.alloc([4, 128], F32)
    wsB = Buf("ws")
    DMA(S, "sp", wsf, D["wsT"].rearrange("g s t -> s g t"), [], [wsB])
    wsm = R2.alloc([4, 128], BF16)
    for g in range(4):
        TT(S, "dve", wsm[:, g, :], wsf[:, g, :], mLQ[:, 0, 128:256], ALU.mult, [wsB, cB], [wsB])
    bsb = R2.alloc([4, 128], F32)
    lng = R2.alloc([512], F32)
    lnb = R2.alloc([512], F32)
    sgB = Buf("sgc")
    DMA(S, "sp", bsb, D["sg_b"].partition_broadcast(128).rearrange("p (g t) -> p g t", g=4), [], [sgB])
    DMA(S, "sp", lng, D["sg_lng"].partition_broadcast(128), [], [sgB])
    DMA(S, "sp", lnb, D["sg_lnb"].partition_broadcast(128), [], [sgB])
    xv = R2.alloc([512], F32); t1 = R2.alloc([512], F32); t2 = R2.alloc([512], F32); zg = R2.alloc([512], F32)
    xu = R2.alloc([512], F32); u1 = R2.alloc([512], F32); u2 = R2.alloc([512], F32); ug = R2.alloc([512], F32)
    zb = R2.alloc([512], BF16)
    st6 = R2.alloc([4, 6], F32); mv = R2.alloc([4, 2], F32); rstd4 = R2.alloc([4], F32)
    osg = [R2.alloc([4, 128], F32) for _ in range(2)]
    osg16 = [R2.alloc([4, 128], BF16) for _ in range(2)]
    osg16B = bufs(2, "osg16")
    xvB, t1B, t2B, zgB, xuB, u1B, u2B, ugB, zbB, stB = (Buf(n) for n in "xv t1 t2 zg xu u1 u2 ug zb st".split())
    osgB = bufs(2, "osg")
    outBs = []
    for ci in range(16):
        tok0 = ci * 128
        tsl = slice(tok0, tok0 + 128)
        tb = tok0 // 512
        ps, psB = next_ps(C)
        for kc in range(8):
            MM(S, ps[:, :], hsg[:, kc, tsl], wpv[:, kc, :], kc == 0, kc == 7, [hsgB[kc][tb], wpvB[kc]], [psB])
        CP(S, "act", xv, ps[:, :], [psB], [xvB])
        gelu_ops(S, zg, xv, t1, t2, xvB, t1B, t2B, zgB)
        for g in range(4):
            S.op("dve", (lambda o, i: (lambda e: e.bn_stats(o, i)))(st6[:, g, :], zg[:, g * 128:(g + 1) * 128]), [zgB], [stB])
            S.op("dve", (lambda o, i: (lambda e: e.bn_aggr(o, i)))(mv[:, g, :], st6[:, g, :]), [stB], [stB])
        ACT(S, rstd4, mv[:, :, 1], AF.Sqrt, [stB], [stB], bias=LN_EPS)
        S.op("dve", lambda e: e.reciprocal(rstd4, rstd4), [stB], [stB])
        for g in range(4):
            TS(S, "dve", zg[:, g * 128:(g + 1) * 128], zg[:, g * 128:(g + 1) * 128], mv[:, g, 0:1], rstd4[:, g:g + 1],
               ALU.subtract, ALU.mult, [zgB, stB], [zgB])
        TT(S, "pool", zg, zg, lng, ALU.mult, [zgB, sgB], [zgB])
        TT(S, "pool", zb, zg, lnb, ALU.add, [zgB, sgB], [zbB])
        pu, puB = next_ps(C)
        for g in range(4):
            for kc in range(8):
                MM(S, pu[:, g * 128:(g + 1) * 128], wpu[:, kc, g * 128:(g + 1) * 128], hsg[:, kc, tsl], kc == 0, kc == 7,
                   [hsgB[kc][tb], wpuB[kc]], [puB])
        CP(S, "act", xu, pu[:, :], [puB], [xuB])
        gelu_ops(S, ug, xu, u1, u2, xuB, u1B, u2B, ugB)
        pz, pzB = next_ps(C)
        for g in range(4):
            MM(S, pz[:, g * 128:(g + 1) * 128], zb[:, g * 128:(g + 1) * 128], wsm[:, g, :], True, True, [zbB, wsB], [pzB])
        o_ = osg[ci % 2]
        oB_ = osgB[ci % 2]
        TT(S, "dve", o_, pz[:, :].rearrange("p (g t) -> p g t", g=4), bsb, ALU.add, [pzB, sgB], [oB_])
        TT(S, "pool", osg16[ci % 2], o_, ug.rearrange("p (g t) -> p g t", g=4), ALU.mult, [oB_, ugB], [osg16B[ci % 2]])
        ob = Buf("out")
        outBs.append(ob)
        DMA(S, "sp", D["yb_blk"](ci), osg16[ci % 2], [osg16B[ci % 2]], [ob])
    S.barrier()
    R2.off = c_mark

    wrw, wrwB = load_w_bf16(C, R2, D["w_rw"], 1024, 1024, "wrw")
    mu = R2.alloc([8], F32)
    vec = R2.alloc([2, 8], F32)
    w2a2 = R2.alloc([256], F32)
    g2 = R2.alloc([256], F32)
    pB = Buf("rwp")
    DMA(S, "sp", mu, D["mu"], [], [pB])
    DMA(S, "sp", vec[:, :, 0:7], D["vec"], [], [pB])
    DMA(S, "sp", w2a2, D["w2a2"], [], [pB])
    DMA(S, "sp", g2, D["g2"], [], [pB])
    for j in range(2):
        TS(S, "dve", vec[:, j, 7:8], vec[:, j, 3:4], -1.0, 1.0, ALU.mult, ALU.add, [pB], [pB])
    V_W0, V_A0, V_KK, V_KA, V_RK, V_GG, V_GB, V_OM = range(8)

    def vv_(j, i):
        return vec[:, j, i:i + 1]

    pbuf = R2.alloc([8, TBR + 1], F32)
    pbB = bufs(8, "pbuf")
    for oc in range(8):
        S.op("pool", (lambda ap: (lambda e: e.memset(ap, 0.0)))(pbuf[:, oc, 0:1]), [], [pbB[oc]])
    xs = R2.alloc([8, TBR], F32)
    xsB = bufs(8, "xs")
    dtmp = R2.alloc([2, TBR], F32)
    dtB = bufs(2, "dtmp")
    tw = R2.alloc([TBR], F32); sgl = R2.alloc([TBR], F32)
    twB, sglB = Buf("tw"), Buf("sgl")
    NTMP = 12
    tmp = [R2.alloc([TBR], F32) for _ in range(NTMP)]
    tmpB = bufs(NTMP, "tmp")
    gbuf = R2.alloc([2, TBR], F32); gbB = bufs(2, "g")
    bonus = R2.alloc([2, TBR], F32); boB = bufs(2, "bonus")
    KR = R2.alloc([2 * 2 * 2, 128], F32)
    KRB = [[Buf(f"KR{j}{c}") for c in range(2)] for j in range(2)]
    Kh = R2.alloc([2, TBR], F32); KhB = bufs(2, "Kh")
    Bh = R2.alloc([2, TBR], F32); BhB = bufs(2, "Bh")
    Kb = R2.alloc([2, TBR], F32); KbB = bufs(2, "Kb")
    Bb = R2.alloc([2, TBR], F32); BbB = bufs(2, "Bb")
    gC = R2.alloc([2, 2], F32); gCB = [[Buf(f"gC{j}{c}") for c in range(2)] for j in range(2)]
    TM = R2.alloc([2 * 2 * 4, 128], F32)
    TMB = [[Buf(f"TM{j}{c}") for c in range(2)] for j in range(2)]
    TK16 = R2.alloc([2 * 2, 128], BF16)
    TK16B = [[Buf(f"TK{j}{c}") for c in range(2)] for j in range(2)]
    cbs = []
    for _c in range(2):
        cbs.append(dict(
            LQk=R2.alloc([4, 256], F32), LQkB=bufs(2, "LQk"), LQb=R2.alloc([4, 256], F32), LQbB=bufs(2, "LQb"),
            Nb=[R2.alloc([4, 128], BF16) for _ in range(2)], NbB=bufs(2, "N"),
            Mb=[R2.alloc([4, 128], BF16) for _ in range(2)], MbB=bufs(2, "M"),
            Pb=R2.alloc([4, 128], BF16), PbB=Buf("P"), Xs=R2.alloc([4, 64], BF16), XsB=Buf("X"),
            Ut=R2.alloc([4, 64], F32), UtB=Buf("Ut"), Wt=R2.alloc([2, 128], F32), WtB=Buf("Wt"),
            Un=R2.alloc([4, 64], F32), UnB=Buf("Un")))
    Ast = [R2.alloc([2, 64], F32) for _ in range(2)]
    AB = [[Buf("A0e"), Buf("A0o")], [Buf("A1e"), Buf("A1o")]]
    S.op("pool", lambda e: e.memset(Ast[0], 0.0), [], [AB[0][0], AB[0][1]])
    yT = R2.alloc([2, TBR], F32); yTB = [[Buf("yTe0"), Buf("yTe1")], [Buf("yTo0"), Buf("yTo1")]]
    yo = [R2.alloc([TBR], BF16) for _ in range(2)]; yoB = bufs(2, "yo")
    PB_A = (0, 1)
    PB_Y = (2, 3)
    rot = [4, 5, 6, 7]
    ri = [0]

    def rps():
        i = rot[ri[0] % 4]
        ri[0] += 1
        return C.ps[i], C.psB[i]

    cur = 0
    for blk_i in range(NBR):
        t0 = blk_i * TBR
        tsl = slice(t0, t0 + TBR)
        tb = t0 // 512
        for oc in range(8):
            ps, psB = rps()
            for kc in range(8):
                MM(S, ps[:, 0:TBR], wrw[:, kc, oc * 128:(oc + 1) * 128], hb[:, kc, tsl], kc == 0, kc == 7,
                   [wrwB[kc], hbB[kc][tb]], [psB])
            CP(S, "act", pbuf[:, oc, 1:TBR + 1], ps[:, 0:TBR], [psB], [pbB[oc]])
            k2 = oc % 2
            TT(S, "dve", dtmp[:, k2, :], pbuf[:, oc, 0:TBR], pbuf[:, oc, 1:TBR + 1], ALU.subtract, [pbB[oc]], [dtB[k2]])
            STT(S, "dve", xs[:, oc, :], dtmp[:, k2, :], mu[:, oc:oc + 1], pbuf[:, oc, 1:TBR + 1], ALU.mult, ALU.add,
                [dtB[k2], pbB[oc], pB], [xsB[oc]])
            CP(S, "act", pbuf[:, oc, 0:1], pbuf[:, oc, TBR:TBR + 1], [pbB[oc]], [pbB[oc]])
        ACT(S, tw[0:64, :], xs[0:64, 6, :], AF.Tanh, [xsB[6]], [twB])
        ACT(S, sgl, xs[:, 7, :], AF.Sigmoid, [xsB[7]], [sglB])
        for j in range(2):
            r_, k_, v_ = xs[:, j, :], xs[:, 2 + j, :], xs[:, 4 + j, :]
            rB_, kB_, vB_ = xsB[j], xsB[2 + j], xsB[4 + j]
            T = lambda i: tmp[i]
            TBf = lambda i: tmpB[i]
            ps, psB = rps()
            MM(S, ps[:, 0:TBR], w2a2[0:64, j * 128:(j + 1) * 128], tw[0:64, :], True, True, [pB, twB], [psB])
            ACT(S, T(0), ps[:, 0:TBR], AF.Sigmoid, [psB, pB], [TBf(0)], bias=vv_(j, V_W0))
            TS(S, "dve", T(0), T(0), NEG_EHALF, None, ALU.mult, None, [TBf(0)], [TBf(0)])
            ps, psB = rps()
            MM(S, ps[:, 0:TBR], w2a2[64:128, j * 128:(j + 1) * 128], xs[64:128, 6, :], True, True, [pB, xsB[6]], [psB])
            ACT(S, T(1), ps[:, 0:TBR], AF.Sigmoid, [psB, pB], [TBf(1)], bias=vv_(j, V_A0))
            ps, psB = rps()
            MM(S, ps[:, 0:TBR], g2[:, j * 128:(j + 1) * 128], sgl, True, True, [pB, sglB], [psB])
            CP(S, "act", gbuf[:, j, :], ps[:, 0:TBR], [psB], [gbB[j]])
            TS(S, "dve", T(2), k_, vv_(j, V_KK), None, ALU.mult, None, [kB_, pB], [TBf(2)])
            TT(S, "pool", T(3), T(2), T(2), ALU.mult, [TBf(2)], [TBf(3)])
            ps, psB = rps()
            MM(S, ps[:, 0:TBR], blk[:, :], T(3), True, True, [cB, TBf(3)], [psB])
            ACT(S, T(3), ps[:, 0:TBR], AF.Sqrt, [psB], [TBf(3)])
            TS(S, "dve", T(3), T(3), 1e-12, None, ALU.max, None, [TBf(3)], [TBf(3)])
            S.op("dve", (lambda o: (lambda e: e.reciprocal(o, o)))(T(3)), [TBf(3)], [TBf(3)])
            TT(S, "pool", T(2), T(2), T(3), ALU.mult, [TBf(2), TBf(3)], [TBf(2)])
            TS(S, "dve", T(4), T(1), vv_(j, V_KA), vv_(j, V_OM), ALU.mult, ALU.add, [TBf(1), pB], [TBf(4)])
            TT(S, "pool", T(4), T(4), k_, ALU.mult, [TBf(4), kB_], [TBf(4)])
            TT(S, "pool", T(5), T(2), T(1), ALU.mult, [TBf(2), TBf(1)], [TBf(5)])
            STT(S, "dve", T(6), r_, vv_(j, V_RK), T(4), ALU.mult, ALU.mult, [rB_, pB, TBf(4)], [TBf(6)])
            ps, psB = rps()
            MM(S, ps[:, 0:TBR], blk[:, :], T(6), True, True, [cB, TBf(6)], [psB])
            TT(S, "dve", bonus[:, j, :], ps[:, 0:TBR], v_, ALU.mult, [psB, vB_], [boB[j]])
            for c in range(2):
                csl = slice(c * CH, (c + 1) * CH)
                S.op("dve", (lambda o, d1: (lambda e: e.tensor_tensor_scan(o, ones[:, :], d1, 0.0, ALU.mult, ALU.add)))(
                    T(7)[:, csl], T(0)[:, csl]), [TBf(0), cB], [TBf(7)])
            TT(S, "pool", T(8), T(7), T(0), ALU.subtract, [TBf(7), TBf(0)], [TBf(8)])
            ACT(S, T(9), T(7), AF.Exp, [TBf(7)], [TBf(9)])
            ACT(S, T(8), T(8), AF.Exp, [TBf(8)], [TBf(8)])
            ACT(S, T(10), T(7), AF.Exp, [TBf(7)], [TBf(10)], scale=-1.0)
            for c in range(2):
                csl = slice(c * CH, (c + 1) * CH)
                last = T(7)[:, (c + 1) * CH - 1:(c + 1) * CH]
                ACT(S, T(11)[:, csl], T(7)[:, csl], AF.Exp, [TBf(7)], [TBf(11)], scale=-1.0, bias=last)
                CP(S, "dve", gC[:, j, c:c + 1], T(9)[:, (c + 1) * CH - 1:(c + 1) * CH], [TBf(9)], [gCB[j][c]])
            krv = KR.rearrange("p (j c w) t -> p j c w t", j=2, c=2)
            TT(S, "dve", krv[:, j, :, 1, :], r_.rearrange("p (c t) -> p c t", c=2), T(9).rearrange("p (c t) -> p c t", c=2),
               ALU.mult, [rB_, TBf(9)], [KRB[j][0], KRB[j][1]])
            TT(S, "pool", krv[:, j, :, 0, :], T(2).rearrange("p (c t) -> p c t", c=2), T(8).rearrange("p (c t) -> p c t", c=2),
               ALU.mult, [TBf(2), TBf(8)], [KRB[j][0], KRB[j][1]])
            TT(S, "dve", Kh[:, j, :], T(4), T(10), ALU.mult, [TBf(4), TBf(10)], [KhB[j]])
            TT(S, "pool", Bh[:, j, :], T(5), T(10), ALU.mult, [TBf(5), TBf(10)], [BhB[j]])
            TT(S, "dve", Kb[:, j, :], T(4), T(11), ALU.mult, [TBf(4), TBf(11)], [KbB[j]])
            TT(S, "pool", Bb[:, j, :], T(5), T(11), ALU.mult, [TBf(5), TBf(11)], [BbB[j]])
            tmv = TM.rearrange("p (j c a) f -> p j c a f", j=2, c=2)
            for c in range(2):
                csl = slice(c * CH, (c + 1) * CH)
                ps, psB = rps()
                TR(S, ps[:, 0:128], v_[:, csl], ident[:, 0, :], [vB_, cB], [psB])
                TR(S, ps[:, 128:256], krv[:, j, c, 0, :], ident[:, 0, :], [KRB[j][c], cB], [psB])
                TR(S, ps[:, 256:384], Kb[:, j, csl], ident[:, 0, :], [KbB[j], cB], [psB])
                TR(S, ps[:, 384:512], Bb[:, j, csl], ident[:, 0, :], [BbB[j], cB], [psB])
                CP(S, "act", tmv[:, j, c, :, :], ps[:, :].rearrange("p (a f) -> p a f", a=4), [psB], [TMB[j][c]])
                CP(S, "act", TK16[:, j * 2 + c, :], ps[:, 128:256], [psB], [TK16B[j][c]])
        krv = KR.rearrange("p (j c w) t -> p j c w t", j=2, c=2)
        tmv = TM.rearrange("p (j c a) f -> p j c a f", j=2, c=2)
        def e3_gen(c):
            csl = slice(c * CH, (c + 1) * CH)
            cb = cbs[c]
            LQk, LQkB, LQb, LQbB, Nb, NbB, Mb, MbB = (cb[k] for k in "LQk LQkB LQb LQbB Nb NbB Mb MbB".split())
            Pb, PbB, Xs, XsB, Ut, UtB, Wt, WtB = (cb[k] for k in "Pb PbB Xs XsB Ut UtB Wt WtB".split())
            lqkv = LQk.rearrange("p (j hh) w -> p hh j w", hh=2)
            lqbv = LQb.rearrange("p (j hh) w -> p hh j w", hh=2)
            nbv = Nb[0].rearrange("p (j hh) w -> p hh j w", hh=2)
            for hh in range(2):
                base = 64 * hh
                pa, paB = rps()
                pb_, pbB_ = rps()
                pc, pcB = rps()
                for j in range(2):
                    rhs = krv[base:base + 64, j, c, :, :].rearrange("p w t -> p (w t)")
                    MM(S, pa[:, j * 256:(j + 1) * 256], Kh[base:base + 64, j, csl], rhs, True, True,
                       [KhB[j], KRB[j][c]], [paB])
                    MM(S, pb_[:, j * 256:(j + 1) * 256], Bh[base:base + 64, j, csl], rhs, True, True,
                       [BhB[j], KRB[j][c]], [pbB_])
                    MM(S, pc[:, j * 128:(j + 1) * 128], krv[base:base + 64, j, c, 0, :], Bh[base:base + 64, j, csl],
                       True, True, [KRB[j][c], BhB[j]], [pcB])
                TT(S, "dve", lqkv[:, hh, :, :], pa[:, :].rearrange("p (a b) -> p a b", a=2), mLQ, ALU.mult,
                   [paB, cB], [LQkB[hh]])
                TT(S, "dve", lqbv[:, hh, :, :], pb_[:, :].rearrange("p (a b) -> p a b", a=2), mLQ, ALU.mult,
                   [pbB_, cB], [LQbB[hh]])
                STT(S, "dve", nbv[:, hh, :, :], pc[:, 0:256].rearrange("p (a b) -> p a b", a=2), -1.0, mSL[:, 0:2, :],
                    ALU.mult, ALU.mult, [pcB, cB], [NbB[0]])
            yield
            n0, m0 = 0, 0
            TS(S, "pool", Mb[0], LQb[:, :, 0:128], -1.0, None, ALU.mult, None, [LQbB[0], LQbB[1]], [MbB[0]])
            TT(S, "pool", Pb, Mb[0], ident, ALU.add, [MbB[0], cB], [PbB])
            for lvl in range(1, 7):
                n1, m1 = 1 - n0, 1 - m0
                pn, pnB = rps()
                for h in range(4):
                    MM(S, pn[:, h * 128:(h + 1) * 128], Mb[m0][:, h, :], Nb[n0][:, h, :], True, True,
                       [MbB[m0], NbB[n0]], [pnB])
                if lvl <= 5:
                    pm, pmB = rps()
                    for h in range(4):
                        MM(S, pm[:, h * 128:(h + 1) * 128], Nb[n0][:, h, :], Mb[m0][:, h, :], True, True,
                           [MbB[m0], NbB[n0]], [pmB])
                yield
                CP(S, "act", Nb[n1], pn[:, :].rearrange("p (a b) -> p a b", a=4), [pnB], [NbB[n1]])
                if lvl <= 5:
                    CP(S, "dve", Mb[m1], pm[:, :].rearrange("p (a b) -> p a b", a=4), [pmB], [MbB[m1]])
                yield
                pp, ppB = rps()
                for h in range(4):
                    MM(S, pp[:, h * 128:(h + 1) * 128], Nb[n1][:, h, :], Pb[:, h, :], True, True, [NbB[n1], PbB], [ppB])
                yield
                TT(S, "dve", Pb, Pb, pp[:, :].rearrange("p (a b) -> p a b", a=4), ALU.add, [PbB, ppB], [PbB])
                n0, m0 = n1, m1
            px, pxB = rps()
            for h in range(4):
                j, base = h // 2, 64 * (h % 2)
                MM(S, px[:, h * 64:(h + 1) * 64], LQk[:, h, 0:128], tmv[:, j, c, 0, base:base + 64], True, True,
                   [LQkB[h % 2], TMB[j][c]], [pxB])
            yield
            CP(S, "act", Xs, px[:, 0:256].rearrange("p (a b) -> p a b", a=4), [pxB], [XsB])
            yield
            pu_, puB_ = rps()
            for h in range(4):
                MM(S, pu_[:, h * 64:(h + 1) * 64], Pb[:, h, :], Xs[:, h, :], True, True, [PbB, XsB], [puB_])
            CP(S, "dve", Ut, pu_[:, 0:256].rearrange("p (a b) -> p a b", a=4), [puB_], [UtB])
            pw, pwB = rps()
            pw2, pw2B = rps()
            for h in range(4):
                j, hh = h // 2, h % 2
                base = 64 * hh
                tgt, tgtB = (pw, pwB) if hh == 0 else (pw2, pw2B)
                MM(S, tgt[base:base + 64, j * 128:(j + 1) * 128], TK16[:, j * 2 + c, base:base + 64], Pb[:, h, :], True, True,
                   [TK16B[j][c], PbB], [tgtB])
            CP(S, "act", Wt[0:64, :, :], pw[0:64, 0:256].rearrange("p (a b) -> p a b", a=2), [pwB], [WtB])
            CP(S, "act", Wt[64:128, :, :], pw2[64:128, 0:256].rearrange("p (a b) -> p a b", a=2), [pw2B], [WtB])

        gens = [e3_gen(0), e3_gen(1)]
        while gens:
            for g_ in list(gens):
                try:
                    next(g_)
                except StopIteration:
                    gens.remove(g_)
        for c in range(2):
            csl = slice(c * CH, (c + 1) * CH)
            cb = cbs[c]
            LQk, LQkB, LQb, LQbB = cb["LQk"], cb["LQkB"], cb["LQb"], cb["LQbB"]
            Ut, UtB, Wt, WtB, Un, UnB = cb["Ut"], cb["UtB"], cb["Wt"], cb["WtB"], cb["Un"], cb["UnB"]
            A0, A1 = Ast[cur], Ast[1 - cur]
            A0B, A1B = AB[cur], AB[1 - cur]
            for h in range(4):
                j, hh = h // 2, h % 2
                base = 64 * hh
                pA, pAB = C.ps[PB_A[hh]], C.psB[PB_A[hh]]
                MM(S, pA[:, j * 64:(j + 1) * 64], Wt[base:base + 64, j, :], A0[base:base + 64, j, :], True, True,
                   [WtB, A0B[hh]], [pAB])
            for hh in range(2):
                pA, pAB = C.ps[PB_A[hh]], C.psB[PB_A[hh]]
                utv = Ut.rearrange("p (j hh) v -> p hh j v", hh=2)
                unv = Un.rearrange("p (j hh) v -> p hh j v", hh=2)
                STT(S, "dve", unv[:, hh, :, :], pA[:, 0:128].rearrange("p (j v) -> p j v", j=2), -1.0, utv[:, hh, :, :],
                    ALU.mult, ALU.subtract, [pAB, UtB], [UnB])
            for h in range(4):
                j, hh = h // 2, h % 2
                base = 64 * hh
                pY, pYB = C.ps[PB_Y[hh]], C.psB[PB_Y[hh]]
                osl = pY[base:base + 64, j * 128:(j + 1) * 128]
                MM(S, osl, A0[base:base + 64, j, :], krv[base:base + 64, j, c, 1, :], True, False, [A0B[hh], KRB[j][c]], [pYB])
                MM(S, osl, tmv[:, j, c, 0, base:base + 64], LQk[:, h, 128:256], False, False, [TMB[j][c], LQkB[hh]], [pYB])
                MM(S, osl, Un[:, h, :], LQb[:, h, 128:256], False, True, [UnB, LQbB[hh]], [pYB])
            for h in range(4):
                j, hh = h // 2, h % 2
                base = 64 * hh
                pA, pAB = C.ps[PB_A[hh]], C.psB[PB_A[hh]]
                osl = pA[base:base + 64, 128 + j * 64:128 + (j + 1) * 64]
                MM(S, osl, tmv[:, j, c, 2, base:base + 64], tmv[:, j, c, 0, base:base + 64], True, False, [TMB[j][c]], [pAB])
                MM(S, osl, tmv[:, j, c, 3, base:base + 64], Un[:, h, :], False, True, [TMB[j][c], UnB], [pAB])
            for h in range(4):
                j, hh = h // 2, h % 2
                base = 64 * hh
                pA, pAB = C.ps[PB_A[hh]], C.psB[PB_A[hh]]
                STT(S, "dve", A1[base:base + 64, j, :], A0[base:base + 64, j, :], gC[base:base + 64, j, c:c + 1],
                    pA[base:base + 64, 128 + j * 64:128 + (j + 1) * 64], ALU.mult, ALU.add,
                    [A0B[hh], gCB[j][c], pAB], [A1B[hh]])
            for hh in range(2):
                base = 64 * hh
                pY, pYB = C.ps[PB_Y[hh]], C.psB[PB_Y[hh]]
                CP(S, "act", yT[base:base + 64, :, csl], pY[base:base + 64, 0:256].rearrange("p (j t) -> p j t", j=2),
                   [pYB], [yTB[hh][c]])
            cur = 1 - cur
        for j in range(2):
            yB_all = [yTB[0][0], yTB[0][1], yTB[1][0], yTB[1][1]]
            ps, psB = rps()
            MM(S, ps[:, 0:TBR], blk64[:, :], yT[:, j, :], True, True, [cB] + yB_all, [psB])
            TT(S, "dve", tmp[0], yT[:, j, :], ps[:, 0:TBR], ALU.subtract, yB_all + [psB], [tmpB[0]])
            TT(S, "pool", tmp[1], tmp[0], tmp[0], ALU.mult, [tmpB[0]], [tmpB[1]])
            ps, psB = rps()
            MM(S, ps[:, 0:TBR], blk64[:, :], tmp[1], True, True, [cB, tmpB[1]], [psB])
            ACT(S, tmp[1], ps[:, 0:TBR], AF.Sqrt, [psB], [tmpB[1]], bias=GN_EPS)
            S.op("dve", (lambda o: (lambda e: e.reciprocal(o, o)))(tmp[1]), [tmpB[1]], [tmpB[1]])
            TT(S, "pool", tmp[0], tmp[0], tmp[1], ALU.mult, [tmpB[0], tmpB[1]], [tmpB[0]])
            ACT(S, tmp[0], tmp[0], AF.Identity, [tmpB[0], pB], [tmpB[0]], bias=vv_(j, V_GB), scale=vv_(j, V_GG))
            TT(S, "pool", tmp[0], tmp[0], bonus[:, j, :], ALU.add, [tmpB[0], boB[j]], [tmpB[0]])
            TT(S, "dve", yo[j], tmp[0], gbuf[:, j, :], ALU.mult, [tmpB[0], gbB[j]], [yoB[j]])
            ob = Buf("out")
            outBs.append(ob)
            DMA(S, "sp", D["ya_blk"](j, blk_i), yo[j], [yoB[j]], [ob])
        if "after_blk" in D:
            D["after_blk"](blk_i, outBs[-2:])
    return outBs


def consts_E():
    s = np.arange(128)[:, None]
    t = np.arange(128)[None, :]
    SU = (s < t).astype(np.float32)
    IU = (s <= t).astype(np.float32)
    SL = (t < s).astype(np.float32)
    blk = np.zeros((128, 128), np.float32)
    blk[:64, :64] = 1.0
    blk[64:, 64:] = 1.0
    return {"ident": np.eye(128, dtype=np.float32), "maskLQ": np.ascontiguousarray(np.concatenate([SU, IU], axis=1)),
            "maskSL": SL, "blk": blk}


def prep_E_weights(ab_w_in, ab_mu, rw_w0, rw_w2, rw_a0, rw_a2, rw_g2, rw_k_k, rw_k_a, rw_r_k, rw_gn_g, rw_gn_b,
                   sg_ln_g, sg_ln_b, sg_ws, sg_b, hs):
    f0, f1 = hs * 256, (hs + 1) * 256
    cols = np.concatenate([np.arange(f0, f1), 512 + np.arange(f0, f1), 1024 + np.arange(f0, f1), np.arange(1536, 1792)])
    w_rw = np.ascontiguousarray(ab_w_in[:, cols])
    mu = np.ascontiguousarray(ab_mu[cols].reshape(8, 128).T)
    w_pu = np.ascontiguousarray(ab_w_in[:, 1792:2304])
    w_pv = np.ascontiguousarray(ab_w_in[:, 2304:2816])
    w2a2 = np.ascontiguousarray(np.concatenate([rw_w2[:, f0:f1], rw_a2[:, f0:f1]], axis=0))
    g2 = np.ascontiguousarray(rw_g2[:, f0:f1])
    vs = [rw_w0, rw_a0, rw_k_k, rw_k_a, rw_r_k.reshape(-1), rw_gn_g, rw_gn_b]
    vec = np.ascontiguousarray(np.stack([v[f0:f1].reshape(2, 128).T for v in vs], axis=2)).astype(np.float32)
    wsT = np.ascontiguousarray(np.transpose(sg_ws, (0, 2, 1)))
    return dict(w_rw=w_rw, mu=mu.astype(np.float32), w_pu=w_pu, w_pv=w_pv, w2a2=w2a2, g2=g2, vec=vec, wsT=wsT,
                sg_b=np.ascontiguousarray(sg_b.reshape(-1)), sg_lng=np.ascontiguousarray(sg_ln_g.reshape(-1)),
                sg_lnb=np.ascontiguousarray(sg_ln_b.reshape(-1)))


E_SHAPES = dict(w_rw=[1024, 1024], mu=[128, 8], w_pu=[1024, 512], w_pv=[1024, 512], w2a2=[128, 256], g2=[128, 256],
                vec=[128, 2, 7], wsT=[4, 128, 128], sg_b=[512], sg_lng=[512], sg_lnb=[512])
O_SHAPES = dict(w_in=[1024, 704], nrm=[128, 4], wq=[256, HPC * 96], wq_sw=[256, HPC * 96], wkv_k=[256, HPC * 64],
                wkv_v=[256, HPC * 64])
P_SHAPES = dict(w_out=[1024, 1024], xa_wq=[1024, 1024], xa_wo=[1024, 1024], xa_wkv=[1024, 2048], lnp=[128, 6, 8],
                w_r=[1024, 36], b_r=[36], moe_wg=[32, 1024, 512], moe_wu=[32, 1024, 512], moe_wd=[32, 512, 1024])
C_SHAPES = dict(ident=[128, 128], maskLQ=[128, 256], maskSL=[128, 128], blk=[128, 128], cosT=[96, SEQ], sinT=[96, SEQ],
                masks=[128, 2048], sel=[32, 32 * 128], par=[128, 2], memT=[1024, 256], x0T=[4, 1024, 512])
PAIRS = [[0, 1], [2, 3], [4, 5], [6, 7]]


def build_fused(n_layers=4):
    nc = bass.Bass("TRN2", target_bir_lowering=False)
    X = {}

    def din(name, shape):
        X[name] = nc.dram_tensor(name, list(shape), F32, kind="ExternalInput").ap()

    for k, v in C_SHAPES.items():
        din(k, v)
    for k, v in E_SHAPES.items():
        din("E_" + k, [2] + v)
    for k, v in O_SHAPES.items():
        din("O_" + k, [2] + v)
    for k, v in P_SHAPES.items():
        din("P_" + k, [4] + v)
    outT = nc.dram_tensor("outT", [4, 1024, 512], F32, kind="ExternalOutput").ap()
    hT_pp = [nc.dram_tensor(f"hT_pp{i}", [4, 1024, 512], F32).ap() for i in range(2)]
    hb16 = nc.dram_tensor("hb16", [4, 1024, 512], BF16).ap()
    hfull16 = nc.dram_tensor("hfull16", [4, 2048, 512], BF16).ap()
    ya16 = nc.dram_tensor("ya16", [2, 256, 2048], BF16).ap()
    ya_g = nc.dram_tensor("ya_g", [2, 512, 2048], BF16).ap()
    o16 = nc.dram_tensor("o16", [4, 128, SEQ], BF16).ap()
    o_g = nc.dram_tensor("o_g", [4, 256, SEQ], BF16).ap()
    yb16 = nc.dram_tensor("yb16", [512, 2048], BF16).ap()
    with contextlib.ExitStack() as st:
        C = make_ctx(nc, st)
        S = C.S
        hb16B = bufs(4, "hb16")
        hfB = bufs(4, "hfull")
        for k in range(4):
            DMA(S, "pool", hb16[k], X["x0T"][k], [], [hb16B[k]])
        S.new_epoch()

        def ag(src, dst, reads, writes):
            S.coll(lambda e: e.collective_compute("AllGather", ALU.bypass, replica_groups=PAIRS, ins=[src], outs=[dst]),
                   reads, writes)

        for k in range(4):
            ag(hb16[k], hfull16[k], [hb16B[k]], [hfB[k]])
        outBs = []
        for layer in range(n_layers):
            j = layer // 2
            D = {n: X[n] for n in C_SHAPES}
            D["h_blk"] = lambda c, tb8: hfull16[tb8 % 4, (tb8 // 4) * 1024 + c * 128:(tb8 // 4) * 1024 + (c + 1) * 128, :]
            D["h_deps"] = (lambda hfB: (lambda tb8: [hfB[tb8 % 4]]))(hfB)
            if layer % 2 == 0:
                for n in E_SHAPES:
                    D[n] = X["E_" + n][j]
                D["hsg_blk"] = lambda c, tb: hb16[tb, c * 128:(c + 1) * 128, :]
                D["ya_blk"] = lambda jj, bi: ya16[bi // 8, jj * 128:(jj + 1) * 128, (bi % 8) * TBR:(bi % 8 + 1) * TBR]
                D["yb_blk"] = lambda ci: yb16.rearrange("(g p) t -> p g t", p=128)[:, :, ci * 128:(ci + 1) * 128]
                xB = bufs(2, "yag")
                acc = []

                def after_blk(bi, bs, xB=xB, acc=acc):
                    acc.extend(bs)
                    if bi % 8 == 7:
                        ag(ya16[bi // 8], ya_g[bi // 8], list(acc), [xB[bi // 8]])
                        del acc[:]
                D["after_blk"] = after_blk
                stage_E(C, D)
                S.new_epoch()
                mix_src = []
                for c in range(8):
                    if c < 4:
                        r, q = c // 2, c % 2
                        mix_src.append(("xchg", (lambda r, q: (lambda tb, half: ya_g[half, r * 256 + q * 128:r * 256 + (q + 1) * 128,
                                                 tb * 512:(tb + 1) * 512]))(r, q), [xB[0], xB[1]]))
                    else:
                        mix_src.append(("local", (lambda c: (lambda tb: yb16[(c - 4) * 128:(c - 3) * 128,
                                                  tb * 512:(tb + 1) * 512]))(c), []))
            else:
                for n in O_SHAPES:
                    D[n] = X["O_" + n][j]
                D["o_blk"] = lambda h, qb: o16[h // 2, (h % 2) * 64:(h % 2) * 64 + 64, qb * 512:(qb + 1) * 512]
                xB = bufs(4, "og")
                acc = []

                def after_head(h, bs, xB=xB, acc=acc):
                    acc.extend(bs)
                    if h % 2 == 1:
                        ag(o16[h // 2], o_g[h // 2], list(acc), [xB[h // 2]])
                        del acc[:]
                D["after_head"] = after_head
                stage_O(C, D)
                S.new_epoch()
                mix_src = []
                for c in range(8):
                    r, q = c // 4, c % 4
                    mix_src.append(("xchg", (lambda r, q: (lambda tb, half: o_g[q, r * 128:(r + 1) * 128,
                                             half * 2048 + tb * 512: half * 2048 + (tb + 1) * 512]))(r, q), [xB[q]]))
            D = {n: X[n] for n in C_SHAPES}
            for n in P_SHAPES:
                D[n] = X["P_" + n][layer]
            h_in = X["x0T"] if layer == 0 else hT_pp[layer % 2]
            h_out = outT if layer == n_layers - 1 else hT_pp[(layer + 1) % 2]
            D["h_blk"] = (lambda h_in: (lambda c, tb: h_in[tb, c * 128:(c + 1) * 128, :]))(h_in)
            D["out_blk"] = (lambda h_out: (lambda c, tb: h_out[tb, c * 128:(c + 1) * 128, :]))(h_out)
            if layer != n_layers - 1:
                D["out16_blk"] = lambda c, tb: hb16[tb, c * 128:(c + 1) * 128, :]
                hfB = bufs(4, "hfull")
                D["after_tb"] = (lambda hfB: (lambda tb, bs: ag(hb16[tb], hfull16[tb], list(bs), [hfB[tb]])))(hfB)
            D["mix_src"] = mix_src
            outBs = stage_P(C, D)
            if layer != n_layers - 1:
                S.new_epoch()
        S.final_wait("sp", outBs)
        S.run()
    return nc


_PROG = {}


def kernel(**inp):
    inp = {k: np.asarray(v) for k, v in inp.items()}
    x, mem = inp["x"], inp["mem"]
    half = SEQ // 2
    if "nc" not in _PROG:
        _PROG["nc"] = build_fused()
    nc = _PROG["nc"]
    cst = {}
    cst.update(consts_E()); cst.update(consts_O()); cst.update(consts_P())
    e_names = ["ab_w_in", "ab_mu", "rw_w0", "rw_w2", "rw_a0", "rw_a2", "rw_g2", "rw_k_k", "rw_k_a", "rw_r_k",
               "rw_gn_g", "rw_gn_b", "sg_ln_g", "sg_ln_b", "sg_ws", "sg_b"]
    shared = {}
    w_out4 = np.stack([inp["ab_w_out"][0], inp["mla_w_out"][0], inp["ab_w_out"][1], inp["mla_w_out"][1]])
    shared["P_w_out"] = np.ascontiguousarray(w_out4)
    shared["P_xa_wq"] = inp["xa_wq"]; shared["P_xa_wo"] = inp["xa_wo"]; shared["P_xa_wkv"] = inp["xa_wkv"]
    shared["P_lnp"] = np.stack([lnp_layout([inp["ln1_g"][l], inp["ln1_b"][l], inp["ln2_g"][l], inp["ln2_b"][l],
                                            inp["ln3_g"][l], inp["ln3_b"][l]]) for l in range(4)])
    shared["P_w_r"] = np.ascontiguousarray(np.concatenate([inp["moe_w_group"], inp["moe_w_expert"]], axis=2))
    shared["P_b_r"] = np.ascontiguousarray(np.concatenate([inp["moe_b_group"], inp["moe_b_expert"]], axis=1))
    shared["P_moe_wg"] = inp["moe_w_gate"]; shared["P_moe_wu"] = inp["moe_w_up"]; shared["P_moe_wd"] = inp["moe_w_down"]
    per_hs = []
    for hs in range(2):
        d = {}
        ew = [prep_E_weights(*[inp[n][j] for n in e_names], hs) for j in range(2)]
        for n in E_SHAPES:
            d["E_" + n] = np.ascontiguousarray(np.stack([ew[j][n] for j in range(2)]))
        ow = [prep_O_weights(inp["mla_w_in"][j], inp["mla_q_norm"][j], inp["mla_kv_norm"][j], inp["mla_wq_b"][j],
                             inp["mla_wkv_b"][j], hs) for j in range(2)]
        for n in O_SHAPES:
            d["O_" + n] = np.ascontiguousarray(np.stack([ow[j][n] for j in range(2)]))
        par = np.zeros((128, 2), np.float32)
        par[:, hs] = 1.0
        d["par"] = par
        per_hs.append(d)
    in_maps = []
    for c in range(NCORES):
        b, s = c // 2, c % 2
        m = dict(cst)
        m.update(shared)
        m.update(per_hs[s])
        xT = x[b, s * half:(s + 1) * half].T
        m["x0T"] = np.ascontiguousarray(xT.reshape(1024, 4, 512).transpose(1, 0, 2))
        m["memT"] = np.ascontiguousarray(mem[b].T)
        in_maps.append({k: np.ascontiguousarray(v, dtype=np.float32) for k, v in m.items()})
    res = run_bass_kernel_spmd(nc, in_maps, core_ids=list(range(NCORES)))
    out = np.empty((x.shape[0], SEQ, 1024), np.float32)
    for c in range(NCORES):
        b, s = c // 2, c % 2
        o = res.results[c]["outT"]
        out[b, s * half:(s + 1) * half, :] = o.transpose(0, 2, 1).reshape(half, 1024)
    return out
```

```python
import contextlib
import numpy as np
import concourse.bass as bass
import concourse.mybir as mybir
from concourse.bass_utils import run_bass_kernel_spmd

F32 = mybir.dt.float32
BF16 = mybir.dt.bfloat16
ALU = mybir.AluOpType
AF = mybir.ActivationFunctionType
AX = mybir.AxisListType

ENGS = ("pe", "act", "dve", "pool", "sp")
ALPHA = float(8 ** 0.25)
LN_EPS = 1e-5
NCORES = 8


class Buf:
    __slots__ = ("name", "w", "r")

    def __init__(self, name=""):
        self.name = name
        self.w = None
        self.r = {}


def bufs(n, name=""):
    return [Buf(f"{name}{i}") for i in range(n)]


class Sched:
    def __init__(self, nc, n_dma_sems=12):
        self.nc = nc
        self.q = {e: [] for e in ENGS}
        self.cnt = {e: 0 for e in ENGS}
        self.known = {e: {} for e in ENGS}
        self.semkeys = list(ENGS)
        self.ekey = {e: e for e in ENGS}
        self.epoch = 0
        self.ndma = n_dma_sems
        self.dma_val = {}
        self.dma_rr = {"sp": 0, "pool": 0, "act": 0}
        for qn in ("sp", "pool", "act"):
            for i in range(n_dma_sems):
                k = f"d_{qn}_{i}"
                self.semkeys.append(k)
                self.dma_val[k] = 0
        self.semkeys.append("cc")
        self.cc_val = 0
        self.n_ops = 0

    def _need(self, eng, reads, writes):
        need = {}

        def add(k, v):
            if need.get(k, 0) < v:
                need[k] = v

        for b in reads:
            if b.w is not None:
                add(b.w[0], b.w[1])
        for b in writes:
            if b.w is not None and not (b.w[2] == "pe" and eng == "pe"):
                add(b.w[0], b.w[1])
            for k, (v, e) in b.r.items():
                add(k, v)
        kn = self.known[eng]
        out = []
        for k, v in need.items():
            if kn.get(k, 0) < v:
                kn[k] = v
                out.append((k, v))
        return out

    def _commit(self, ev, reads, writes):
        k, v, e = ev
        for b in reads:
            if b.r.get(k, (0, e))[0] <= v:
                b.r[k] = (v, e)
        for b in writes:
            b.w = ev
            b.r = {}

    def op(self, eng, fn, reads=(), writes=(), sig=True):
        waits = self._need(eng, reads, writes)
        if sig:
            self.cnt[eng] += 1
            self.q[eng].append((waits, fn, (self.ekey[eng], 1)))
            v = self.cnt[eng]
        else:
            self.q[eng].append((waits, fn, None))
            v = self.cnt[eng] + 1
        self._commit((self.ekey[eng], v, eng), reads, writes)
        self.n_ops += 1

    def dma(self, queue, fn, reads=(), writes=()):
        i = self.dma_rr[queue]
        self.dma_rr[queue] = (i + 1) % self.ndma
        k = f"d_{queue}_{i}"
        waits = self._need(queue, reads, writes)
        prev = self.dma_val[k]
        kn = self.known[queue]
        if prev > 0 and kn.get(k, 0) < prev:
            kn[k] = prev
            waits.append((k, prev))
        self.dma_val[k] = prev + 16
        self.q[queue].append((waits, fn, (k, 16)))
        self._commit((k, prev + 16, "dma"), reads, writes)
        self.n_ops += 1

    def coll(self, fn, reads=(), writes=()):
        waits = self._need("pool", reads, writes)
        kn = self.known["pool"]
        if self.cc_val > 0 and kn.get("cc", 0) < self.cc_val:
            kn["cc"] = self.cc_val
            waits.append(("cc", self.cc_val))
        self.cc_val += 1
        self.q["pool"].append((waits, fn, ("cc", 1)))
        self._commit(("cc", self.cc_val, "dma"), reads, writes)
        self.n_ops += 1

    def barrier(self):
        cur = {self.ekey[e]: self.cnt[e] for e in ENGS}
        cur.update(self.dma_val)
        cur["cc"] = self.cc_val
        for eng in ENGS:
            kn = self.known[eng]
            waits = []
            for k, v in cur.items():
                if v > 0 and kn.get(k, 0) < v:
                    kn[k] = v
                    waits.append((k, v))
            if waits:
                self.q[eng].append((waits, None, None))

    def new_epoch(self):
        self.barrier()
        self.epoch += 1
        for e in ENGS:
            self.ekey[e] = f"{e}_{self.epoch}"
            self.semkeys.append(self.ekey[e])
            self.cnt[e] = 0

    def final_wait(self, eng, bufs_):
        waits = self._need(eng, bufs_, ())
        self.q[eng].append((waits, None, None))

    def run(self):
        nc = self.nc
        with contextlib.ExitStack() as st:
            sems = {}
            for k in self.semkeys:
                sems[k] = st.enter_context(nc.semaphore(k))
            block = st.enter_context(nc.Block())

            def replay(engname):
                def body(e):
                    for waits, fn, inc in self.q[engname]:
                        for (k, v) in waits:
                            e.wait_ge(sems[k], v)
                        if fn is not None:
                            ins = fn(e)
                            if inc is not None:
                                ins.then_inc(sems[inc[0]], inc[1])
                return body

            block.tensor(replay("pe"))
            block.scalar(replay("act"))
            block.vector(replay("dve"))
            block.gpsimd(replay("pool"))
            block.sync(replay("sp"))


class Region:
    def __init__(self, arena, start, size):
        self.arena, self.start, self.size, self.off = arena, start, size, 0

    def reset(self):
        self.off = 0

    def alloc(self, shape, dtype, parts=128):
        n = int(np.prod(shape))
        isz = 4 if dtype == F32 else 2
        nb = (n * isz + 63) // 64 * 64
        assert self.off + nb <= self.size, (self.off, nb, self.size)
        a = (self.start + self.off) // 4
        ap = self.arena[0:parts, a:a + nb // 4]
        self.off += nb
        if dtype != F32:
            ap = ap.bitcast(dtype)
        ap = ap[:, 0:n]
        if len(shape) == 2:
            ap = ap.rearrange("p (a b) -> p a b", a=shape[0])
        elif len(shape) == 3:
            ap = ap.rearrange("p (a b c) -> p a b c", a=shape[0], b=shape[1])
        return ap


class Ctx:
    pass


def make_ctx(nc, st, arena_bytes=206 * 1024):
    C = Ctx()
    C.nc = nc
    C.S = Sched(nc)
    C.arena = st.enter_context(nc.sbuf_tensor("arena", [128, arena_bytes // 4], F32))
    C.arena_bytes = arena_bytes
    C.ps = [st.enter_context(nc.psum_tensor(f"ps{i}", [128, 512], F32)) for i in range(8)]
    C.psB = bufs(8, "ps")
    C.ps_rr = 0
    return C


def next_ps(C):
    i = C.ps_rr
    C.ps_rr = (i + 1) % 8
    return C.ps[i], C.psB[i]


def MM(S, out, lhsT, rhs, start, stop, reads, writes):
    S.op("pe", lambda e: e.matmul(out, lhsT, rhs, start=start, stop=stop), reads, writes)


def TR(S, out, in_, ident, reads, writes):
    S.op("pe", lambda e: e.transpose(out, in_, ident), reads, writes)


def ACT(S, out, in_, func, reads, writes, bias=0.0, scale=1.0, accum_out=None):
    if accum_out is None:
        S.op("act", lambda e: e.activation(out=out, in_=in_, func=func, bias=bias, scale=scale), reads, writes)
    else:
        S.op("act", lambda e: e.activation(out=out, in_=in_, func=func, bias=bias, scale=scale,
                                           accum_out=accum_out), reads, writes)


def TT(S, eng, out, in0, in1, op, reads, writes):
    S.op(eng, lambda e: e.tensor_tensor(out, in0, in1, op), reads, writes)


def TS(S, eng, out, in0, s1, s2, op0, op1, reads, writes):
    if s2 is None:
        S.op(eng, lambda e: e.tensor_scalar(out, in0, s1, None, op0), reads, writes)
    else:
        S.op(eng, lambda e: e.tensor_scalar(out, in0, s1, s2, op0, op1), reads, writes)


def STT(S, eng, out, in0, scalar, in1, op0, op1, reads, writes):
    S.op(eng, lambda e: e.scalar_tensor_tensor(out, in0, scalar, in1, op0, op1), reads, writes)


def CP(S, eng, out, in_, reads, writes):
    if eng == "act":
        S.op("act", lambda e: e.copy(out, in_), reads, writes)
    else:
        S.op(eng, lambda e: e.tensor_copy(out, in_), reads, writes)


def DMA(S, q, out, in_, reads, writes):
    S.dma(q, lambda e: e.dma_start(out=out, in_=in_), reads, writes)


def load_w_bf16(C, reg, dram_ap, K, N, name, n0=0, n1=None):
    n1 = N if n1 is None else n1
    kc = K // 128
    t = reg.alloc([kc, n1 - n0], BF16)
    B = bufs(kc, name)
    src = dram_ap.rearrange("(c p) n -> p c n", p=128)
    for c in range(kc):
        DMA(C.S, "pool", t[:, c, :], src[:, c, n0:n1], [], [B[c]])
    return t, B


def ln_block(C, reg, reg2, z, zB, T, g_ap, b_ap, emit_out):
    S = C.S
    zb = reg.alloc([8, T], BF16)
    zs = reg.alloc([8, T], BF16)
    zbB, zsB = bufs(8, "zb"), bufs(8, "zs")
    for c in range(8):
        CP(S, "pool", zb[:, c, :], z[:, c, :], [zB[c]], [zbB[c]])
        ACT(S, zs[:, c, :], z[:, c, :], AF.Square, [zB[c]], [zsB[c]])
    p1, p1B = next_ps(C)
    p2, p2B = next_ps(C)
    for c in range(8):
        MM(S, p1[:, 0:T], C.ones_m[:, :], zb[:, c, :], c == 0, c == 7, [zbB[c], C.onesB], [p1B])
    for c in range(8):
        MM(S, p2[:, 0:T], C.ones_m[:, :], zs[:, c, :], c == 0, c == 7, [zsB[c], C.onesB], [p2B])
    mean = reg2.alloc([T], F32)
    rstd = reg2.alloc([T], F32)
    nmr = reg2.alloc([T], F32)
    mB, rB, nB = Buf("mean"), Buf("rstd"), Buf("nmr")
    CP(S, "act", mean, p1[:, 0:T], [p1B], [mB])
    STT(S, "dve", rstd, mean, -1.0, mean, ALU.mult, ALU.mult, [mB], [rB])
    TT(S, "dve", rstd, rstd, p2[:, 0:T], ALU.add, [rB, p2B], [rB])
    ACT(S, rstd, rstd, AF.Sqrt, [rB], [rB], bias=LN_EPS)
    S.op("dve", lambda e: e.reciprocal(rstd, rstd), [rB], [rB])
    STT(S, "dve", nmr, mean, -1.0, rstd, ALU.mult, ALU.mult, [mB, rB], [nB])
    t = reg2.alloc([2, T], F32)
    tB = bufs(2, "lnt")
    for c in range(8):
        k = c % 2
        TT(S, "dve", t[:, k, :], z[:, c, :], rstd, ALU.mult, [zB[c], rB], [tB[k]])
        TT(S, "pool", t[:, k, :], t[:, k, :], nmr, ALU.add, [tB[k], nB], [tB[k]])
        ACT(S, t[:, k, :], t[:, k, :], AF.Identity, [tB[k], C.lnpB], [tB[k]],
            bias=b_ap[:, c:c + 1], scale=g_ap[:, c:c + 1])
        emit_out(c, t[:, k, :], tB[k])


NT = 2048
TB = 512
NTB = NT // TB
NEXP = 32


def stage_P(C, D, n_exp=NEXP):
    S = C.S
    KB = 1024
    R_h = Region(C.arena, 0, 64 * KB)
    R_hb = Region(C.arena, 64 * KB, 32 * KB)
    R_y = Region(C.arena, 96 * KB, 64 * KB)
    R_t = Region(C.arena, 160 * KB, C.arena_bytes - 160 * KB)
    hres = R_h.alloc([8, NT], F32)
    hb = R_hb.alloc([8, NT], BF16)
    hresB = [bufs(NTB, f"hres{c}_") for c in range(8)]
    hbB = [bufs(NTB, f"hb{c}_") for c in range(8)]

    C.ones_m = R_t.alloc([128], BF16)
    C.onesB = Buf("ones")
    S.op("pool", lambda e: e.memset(C.ones_m, 1.0 / 1024.0), [], [C.onesB])
    ones1 = R_t.alloc([128], BF16)
    ones1B = Buf("ones1")
    S.op("pool", lambda e: e.memset(ones1, 1.0), [], [ones1B])
    lnp = R_t.alloc([6, 8], F32)
    C.lnpB = Buf("lnp")
    DMA(S, "sp", lnp, D["lnp"], [], [C.lnpB])
    ident = R_t.alloc([128], F32)
    identB = Buf("ident")
    DMA(S, "sp", ident, D["ident"], [], [identB])
    par = R_t.alloc([2], F32)
    parB = Buf("par")
    DMA(S, "sp", par, D["par"], [], [parB])
    kT = R_t.alloc([8, 256], BF16)
    vv = R_t.alloc([2, 1024], BF16)
    kTB, vvB = Buf("kT"), Buf("vv")
    t_mark = R_t.off

    for c in range(8):
        for tb in range(NTB):
            DMA(S, "sp", hres[:, c, tb * TB:(tb + 1) * TB], D["h_blk"](c, tb), D.get("h_deps", []), [hresB[c][tb]])

    memT, memB = load_w_bf16(C, R_y, D["memT"], 1024, 256, "memT")
    wk_, wkB_ = load_w_bf16(C, R_y, D["xa_wkv"], 1024, 2048, "wkvk", 0, 1024)
    wv_, wvB_ = load_w_bf16(C, R_y, D["xa_wkv"], 1024, 2048, "wkvv", 1024, 2048)
    for oc in range(8):
        ps, psB = next_ps(C)
        for kc in range(8):
            MM(S, ps[:, 0:256], wk_[:, kc, oc * 128:(oc + 1) * 128], memT[:, kc, :], kc == 0, kc == 7,
               [wkB_[kc], memB[kc]], [psB])
        CP(S, "act", kT[:, oc, :], ps[:, 0:256], [psB], [kTB])
    for mc in range(2):
        for nh in range(2):
            ps, psB = next_ps(C)
            for kc in range(8):
                MM(S, ps[:, :], memT[:, kc, mc * 128:(mc + 1) * 128], wv_[:, kc, nh * 512:(nh + 1) * 512],
                   kc == 0, kc == 7, [wvB_[kc], memB[kc]], [psB])
            CP(S, "act", vv[:, mc, nh * 512:(nh + 1) * 512], ps[:, :], [psB], [vvB])
    S.barrier()
    R_y.reset()

    wo_m, wo_mB = load_w_bf16(C, R_y, D["w_out"], 1024, 1024, "w_out")
    R_t.off = t_mark
    bl_a = R_t.alloc([8, TB], BF16)
    bl_b = R_t.alloc([8, TB], BF16)
    blB = [bufs(8, "bla"), bufs(8, "blb")]
    t_markA = R_t.off
    mxs = [R_y.alloc([8, TB], BF16) for _ in range(2)]
    mxB = [Buf("mx0"), Buf("mx1")]
    z = R_y.alloc([8, TB], F32)
    zB = bufs(8, "z")
    ln_mark = R_y.off
    for tb in range(NTB):
        tsl = slice(tb * TB, (tb + 1) * TB)
        mx, mB_ = mxs[tb % 2], mxB[tb % 2]
        for c in range(8):
            kind, fn, deps = D["mix_src"][c]
            if kind == "local":
                DMA(S, "sp", mx[:, c, :], fn(tb), deps, [mB_])
            else:
                DMA(S, "sp", bl_a[:, c, :], fn(tb, 0), deps, [blB[0][c]])
                DMA(S, "sp", bl_b[:, c, :], fn(tb, 1), deps, [blB[1][c]])
                TS(S, "dve", bl_a[:, c, :], bl_a[:, c, :], par[:, 0:1], None, ALU.mult, None, [blB[0][c], parB], [blB[0][c]])
                STT(S, "dve", mx[:, c, :], bl_b[:, c, :], par[:, 1:2], bl_a[:, c, :], ALU.mult, ALU.add,
                    [blB[1][c], blB[0][c], parB], [mB_])
        for oc in range(8):
            ps, psB = next_ps(C)
            for kc in range(8):
                MM(S, ps[:, :], wo_m[:, kc, oc * 128:(oc + 1) * 128], mx[:, kc, :], kc == 0, kc == 7,
                   [wo_mB[kc], mB_], [psB])
            STT(S, "dve", z[:, oc, :], hres[:, oc, tsl], ALPHA, ps[:, :], ALU.mult, ALU.add,
                [hresB[oc][tb], psB], [zB[oc]])
        R_y.off = ln_mark
        R_t.off = t_markA

        def out1(c, src, srcB, tb=tb, tsl=tsl):
            CP(S, "dve", hres[:, c, tsl], src, [srcB], [hresB[c][tb]])
            CP(S, "pool", hb[:, c, tsl], src, [srcB], [hbB[c][tb]])
        ln_block(C, R_y, R_t, z, zB, TB, lnp[:, 0, :], lnp[:, 1, :], out1)
    S.barrier()
    R_y.reset()

    wq, wqB = load_w_bf16(C, R_y, D["xa_wq"], 1024, 1024, "wq")
    wo, woB = load_w_bf16(C, R_y, D["xa_wo"], 1024, 1024, "wo")
    R_t.off = t_mark
    qT = R_t.alloc([8, TB], BF16)
    qTB = bufs(8, "qT")
    oT = R_t.alloc([8, TB], BF16)
    oTB = bufs(8, "oT")
    pT = R_t.alloc([4, TB], BF16)
    pTB = [bufs(2, "pTa"), bufs(2, "pTb")]
    rden = R_t.alloc([2, TB], F32)
    t_mark2 = R_t.off
    rdB = bufs(2, "rden")
    z = R_y.alloc([8, TB], F32)
    zB = bufs(8, "z2")
    ln_mark = R_y.off
    for tb in range(NTB):
        tsl = slice(tb * TB, (tb + 1) * TB)
        for oc in range(8):
            ps, psB = next_ps(C)
            for kc in range(8):
                MM(S, ps[:, :], wq[:, kc, oc * 128:(oc + 1) * 128], hb[:, kc, tsl], kc == 0, kc == 7,
                   [wqB[kc], hbB[kc][tb]], [psB])
            ACT(S, qT[:, oc, :], ps[:, :], AF.Copy, [psB], [qTB[oc]], scale=1.0 / 16.0)
        for hh in range(4):
            pb = hh % 2
            for mc in range(2):
                ps, psB = next_ps(C)
                for j in range(2):
                    MM(S, ps[:, :], kT[:, 2 * hh + j, mc * 128:(mc + 1) * 128], qT[:, 2 * hh + j, :],
                       j == 0, j == 1, [kTB, qTB[2 * hh + j]], [psB])
                ACT(S, pT[:, pb * 2 + mc, :], ps[:, :], AF.Exp, [psB], [pTB[pb][mc]])
            dps, dpsB = next_ps(C)
            for mc in range(2):
                MM(S, dps[:, :], ones1[:, :], pT[:, pb * 2 + mc, :], mc == 0, mc == 1,
                   [ones1B, pTB[pb][mc]], [dpsB])
            S.op("dve", (lambda o, i: (lambda e: e.reciprocal(o, i)))(rden[:, pb, :], dps[:, :]), [dpsB], [rdB[pb]])
            for j in range(2):
                ps, psB = next_ps(C)
                for mc in range(2):
                    MM(S, ps[:, :], vv[:, mc, hh * 256 + j * 128: hh * 256 + (j + 1) * 128], pT[:, pb * 2 + mc, :],
                       mc == 0, mc == 1, [vvB, pTB[pb][mc]], [psB])
                TT(S, "dve", oT[:, 2 * hh + j, :], ps[:, :], rden[:, pb, :], ALU.mult, [psB, rdB[pb]], [oTB[2 * hh + j]])
        for oc in range(8):
            ps, psB = next_ps(C)
            for kc in range(8):
                MM(S, ps[:, :], wo[:, kc, oc * 128:(oc + 1) * 128], oT[:, kc, :], kc == 0, kc == 7,
                   [woB[kc], oTB[kc]], [psB])
            STT(S, "dve", z[:, oc, :], hres[:, oc, tsl], ALPHA, ps[:, :], ALU.mult, ALU.add,
                [hresB[oc][tb], psB], [zB[oc]])
        R_y.off = ln_mark
        R_t.off = t_mark2

        def out2(c, src, srcB, tb=tb, tsl=tsl):
            CP(S, "dve", hres[:, c, tsl], src, [srcB], [hresB[c][tb]])
            CP(S, "pool", hb[:, c, tsl], src, [srcB], [hbB[c][tb]])
        ln_block(C, R_y, R_t, z, zB, TB, lnp[:, 2, :], lnp[:, 3, :], out2)
    S.barrier()
    R_y.reset()

    BIG = 1.0e4
    R_t.off = t_mark
    wr = R_t.alloc([8, 36], F32)
    wrB = Buf("wr")
    DMA(S, "sp", wr, D["w_r"].rearrange("(c p) n -> p c n", p=128), [], [wrB])
    brb = R_t.alloc([36], F32)
    brB = Buf("br")
    DMA(S, "sp", brb, D["b_r"].partition_broadcast(128), [], [brB])
    sel = R_t.alloc([32, 128], F32, parts=32)
    selB = Buf("sel")
    DMA(S, "sp", sel, D["sel"].rearrange("k (e p) -> k e p", p=128), [], [selB])
    GT = R_t.alloc([NT], F32, parts=32)
    GTB = bufs(NTB, "GT")
    NTT = NT // 128
    rt = R_y
    L = rt.alloc([NTT, 36], F32)
    Em = rt.alloc([NTT, 32], F32)
    oh1 = rt.alloc([NTT, 32], F32)
    oh2 = rt.alloc([NTT, 32], F32)
    G = rt.alloc([NTT, 32], F32)
    sm = rt.alloc([NTT, 16], F32)
    tB_ = [Buf(f"rt{i}") for i in range(NTT)]
    for tt in range(NTT):
        tb = tt // 4
        B_ = tB_[tt]
        ps, psB = next_ps(C)
        for kc in range(8):
            MM(S, ps[:, 0:36], hres[:, kc, tt * 128:(tt + 1) * 128], wr[:, kc, :], kc == 0, kc == 7,
               [hresB[kc][tb], wrB], [psB])
        Lt, Et, o1, o2, Gt, s = L[:, tt, :], Em[:, tt, :], oh1[:, tt, :], oh2[:, tt, :], G[:, tt, :], sm[:, tt, :]
        TT(S, "dve", Lt, ps[:, 0:36], brb, ALU.add, [psB, brB], [B_])
        S.op("dve", (lambda o, i: (lambda e: e.reduce_max(o, i, AX.X)))(s[:, 0:1], Lt[:, 0:4]), [B_], [B_])
        TS(S, "dve", s[:, 4:8], Lt[:, 0:4], s[:, 0:1], None, ALU.is_equal, ALU.bypass, [B_], [B_])
        TS(S, "dve", s[:, 4:8], s[:, 4:8], BIG, -BIG, ALU.mult, ALU.add, [B_], [B_])
        TS(S, "dve", s[:, 1:2], s[:, 0:1], -1.0, None, ALU.mult, ALU.bypass, [B_], [B_])
        ACT(S, s[:, 8:12], Lt[:, 0:4], AF.Exp, [B_], [B_], bias=s[:, 1:2], accum_out=s[:, 2:3])
        S.op("dve", (lambda o, i: (lambda e: e.reciprocal(o, i)))(s[:, 3:4], s[:, 2:3]), [B_], [B_])
        for g in range(4):
            TS(S, "dve", Et[:, g * 8:(g + 1) * 8], Lt[:, 4 + g * 8: 12 + g * 8], s[:, 4 + g:5 + g], None,
               ALU.add, ALU.bypass, [B_], [B_])
        S.op("dve", (lambda o, i: (lambda e: e.reduce_max(o, i, AX.X)))(s[:, 12:13], Et), [B_], [B_])
        TS(S, "dve", o1, Et, s[:, 12:13], None, ALU.is_equal, ALU.bypass, [B_], [B_])
        STT(S, "dve", Et, o1, -BIG, Et, ALU.mult, ALU.add, [B_], [B_])
        S.op("dve", (lambda o, i: (lambda e: e.reduce_max(o, i, AX.X)))(s[:, 13:14], Et), [B_], [B_])
        TS(S, "dve", o2, Et, s[:, 13:14], None, ALU.is_equal, ALU.bypass, [B_], [B_])
        TS(S, "dve", s[:, 14:15], s[:, 12:13], -1.0, None, ALU.mult, ALU.bypass, [B_], [B_])
        ACT(S, s[:, 15:16], s[:, 13:14], AF.Exp, [B_], [B_], bias=s[:, 14:15])
        TS(S, "dve", s[:, 14:15], s[:, 15:16], 1.0, None, ALU.add, ALU.bypass, [B_], [B_])
        S.op("dve", (lambda o, i: (lambda e: e.reciprocal(o, i)))(s[:, 14:15], s[:, 14:15]), [B_], [B_])
        TT(S, "dve", s[:, 14:15], s[:, 14:15], s[:, 3:4], ALU.mult, [B_], [B_])
        TT(S, "dve", s[:, 15:16], s[:, 15:16], s[:, 14:15], ALU.mult, [B_], [B_])
        TS(S, "dve", Gt, o1, s[:, 14:15], None, ALU.mult, ALU.bypass, [B_], [B_])
        STT(S, "dve", Gt, o2, s[:, 15:16], Gt, ALU.mult, ALU.add, [B_], [B_])
        tp, tpB = next_ps(C)
        TR(S, tp[0:32, 0:128], Gt, ident[:, :], [B_, identB], [tpB])
        CP(S, "act", GT[:, tt * 128:(tt + 1) * 128], tp[0:32, 0:128], [tpB], [GTB[tb]])
    S.barrier()
    R_y.reset()

    yacc = R_y.alloc([8, NT], F32)
    yB = [bufs(NTB, f"y{c}_") for c in range(8)]
    for c in range(8):
        for tb in range(NTB):
            tsl = slice(tb * TB, (tb + 1) * TB)
            ACT(S, yacc[:, c, tsl], hres[:, c, tsl], AF.Copy, [hresB[c][tb]], [yB[c][tb]], scale=ALPHA)
    S.barrier()
    R_h.reset()
    wbuf = []
    for i in range(2):
        wg = R_h.alloc([8, 512], BF16)
        wu = R_h.alloc([8, 512], BF16)
        wd = R_h.alloc([4, 1024], BF16)
        wbuf.append((wg, wu, wd, bufs(8, f"wg{i}"), bufs(8, f"wu{i}"), bufs(4, f"wd{i}")))
    sg = [R_h.alloc([TB], F32) for _ in range(2)]
    sgB = bufs(2, "sg")
    tt_ = [R_h.alloc([TB], F32) for _ in range(2)]
    ttB = bufs(2, "tt")
    gbs = [R_h.alloc([TB], F32) for _ in range(2)]
    gbB = bufs(2, "gb")
    hg = [R_t.alloc([4, TB], BF16) for _ in range(2)]
    hgB = [bufs(4, "hga"), bufs(4, "hgb")]
    P_G, P_U, P_D, P_B = (0, 1), (2, 3), (4, 5), (6, 7)
    wgs = D["moe_wg"]
    wus = D["moe_wu"]
    wds = D["moe_wd"]

    def load_expert(e):
        wg, wu, wd, bg, bu, bd = wbuf[e % 2]
        sg_ = wgs[e].rearrange("(c p) n -> p c n", p=128)
        su_ = wus[e].rearrange("(c p) n -> p c n", p=128)
        sd_ = wds[e].rearrange("(c p) n -> p c n", p=128)
        for c in range(0, 8, 2):
            DMA(S, "pool", wg[:, c:c + 2, :], sg_[:, c:c + 2, :], [], [bg[c], bg[c + 1]])
        for c in range(0, 8, 2):
            DMA(S, "pool", wu[:, c:c + 2, :], su_[:, c:c + 2, :], [], [bu[c], bu[c + 1]])
        for c in range(4):
            DMA(S, "pool", wd[:, c, :], sd_[:, c, :], [], [bd[c]])

    def down_part(e, tb, it):
        wg, wu, wd, bg, bu, bd = wbuf[e % 2]
        tsl = slice(tb * TB, (tb + 1) * TB)
        hgi, hgBi = hg[it % 2], hgB[it % 2]
        for dc in range(8):
            pi = P_D[dc % 2]
            ps, psB = C.ps[pi], C.psB[pi]
            for f in range(4):
                MM(S, ps[:, :], wd[:, f, dc * 128:(dc + 1) * 128], hgi[:, f, :], f == 0, f == 3,
                   [bd[f], hgBi[f]], [psB])
            TT(S, "dve", yacc[:, dc, tsl], yacc[:, dc, tsl], ps[:, :], ALU.add, [yB[dc][tb], psB], [yB[dc][tb]])

    load_expert(0)
    it = 0
    prev = None
    for e in range(n_exp):
        wg, wu, wd, bg, bu, bd = wbuf[e % 2]
        for tb in range(NTB):
            tsl = slice(tb * TB, (tb + 1) * TB)
            k2 = it % 2
            pbi = P_B[k2]
            MM(S, C.ps[pbi][:, :], sel[:, e, :], GT[:, tsl], True, True, [selB, GTB[tb]], [C.psB[pbi]])
            CP(S, "act", gbs[k2], C.ps[pbi][:, :], [C.psB[pbi]], [gbB[k2]])
            hgi, hgBi = hg[k2], hgB[k2]
            for f in range(4):
                pg, pu = P_G[f % 2], P_U[f % 2]
                for kc in range(8):
                    MM(S, C.ps[pg][:, :], wg[:, kc, f * 128:(f + 1) * 128], hb[:, kc, tsl], kc == 0, kc == 7,
                       [bg[kc], hbB[kc][tb]], [C.psB[pg]])
                for kc in range(8):
                    MM(S, C.ps[pu][:, :], wu[:, kc, f * 128:(f + 1) * 128], hb[:, kc, tsl], kc == 0, kc == 7,
                       [bu[kc], hbB[kc][tb]], [C.psB[pu]])
                f2 = f % 2
                ACT(S, sg[f2], C.ps[pg][:, :], AF.Silu, [C.psB[pg]], [sgB[f2]])
                TT(S, "dve", tt_[f2], C.ps[pu][:, :], gbs[k2], ALU.mult, [C.psB[pu], gbB[k2]], [ttB[f2]])
                TT(S, "dve", hgi[:, f, :], tt_[f2], sg[f2], ALU.mult, [ttB[f2], sgB[f2]], [hgBi[f]])
            if prev is not None:
                down_part(*prev)
            if tb == 0 and e + 1 < n_exp:
                load_expert(e + 1)
            prev = (e, tb, it)
            it += 1
    down_part(*prev)
    S.barrier()
    R_h.reset()

    outBs = []
    stg16 = [R_h.alloc([TB], BF16) for _ in range(4)]
    stg16B = bufs(4, "stg16")
    stg = [R_h.alloc([TB], F32) for _ in range(4)]
    stgB = bufs(4, "stg")
    cnt = [0]
    ln_mark = R_h.off
    for tb in range(NTB):
        tsl = slice(tb * TB, (tb + 1) * TB)
        R_h.off = ln_mark

        def out3(c, src, srcB, tb=tb, tsl=tsl):
            i = cnt[0] % 4
            cnt[0] += 1
            CP(S, "dve", stg[i], src, [srcB], [stgB[i]])
            ob = Buf("out")
            outBs.append(ob)
            DMA(S, "sp", D["out_blk"](c, tb), stg[i], [stgB[i]], [ob])
            if "out16_blk" in D:
                CP(S, "pool", stg16[i], src, [srcB], [stg16B[i]])
                ob = Buf("out16")
                outBs.append(ob)
                DMA(S, "sp", D["out16_blk"](c, tb), stg16[i], [stg16B[i]], [ob])
        n_before = len(outBs)
        ln_block(C, R_h, R_h, yacc[:, :, tsl], [yB[c][tb] for c in range(8)], TB, lnp[:, 4, :], lnp[:, 5, :], out3)
        if "after_tb" in D:
            D["after_tb"](tb, outBs[n_before:])
    return outBs


def consts_P():
    sel = np.zeros((32, 32, 128), np.float32)
    for e in range(32):
        sel[e, e, :] = 1.0
    return {"sel": sel.reshape(32, 32 * 128), "ident": np.eye(128, dtype=np.float32)}


def lnp_layout(vs):
    return np.ascontiguousarray(np.stack([v.reshape(8, 128).T for v in vs], axis=1)).astype(np.float32)


SEQ = 4096
NQB = SEQ // 512
RMS_EPS = 1e-6
HPC = 8


def stage_O(C, D):
    S = C.S
    KB = 1024
    R1 = Region(C.arena, 0, 64 * KB)
    R2 = Region(C.arena, 64 * KB, 100 * KB)
    R3 = Region(C.arena, 164 * KB, C.arena_bytes - 164 * KB)
    cqn = R2.alloc([2, SEQ], BF16)
    ckvn = R2.alloc([2, SEQ], BF16)
    kpeT = R2.alloc([SEQ], BF16)
    cosT = R2.alloc([SEQ], F32)
    sinT = R2.alloc([SEQ], F32)
    cqnB = [bufs(NQB, "cqn0_"), bufs(NQB, "cqn1_")]
    ckvB = [bufs(NQB, "ckv0_"), bufs(NQB, "ckv1_")]
    kpeB = bufs(NQB, "kpe")
    tabB = Buf("tab")
    DMA(S, "sp", cosT[0:96, :], D["cosT"], [], [tabB])
    DMA(S, "sp", sinT[0:96, :], D["sinT"], [], [tabB])
    nrm = R2.alloc([4], F32)
    nrmB = Buf("nrm")
    DMA(S, "sp", nrm, D["nrm"], [], [nrmB])
    onesm = R2.alloc([128], BF16)
    onesB = Buf("ones")
    S.op("pool", lambda e: e.memset(onesm, 1.0 / 256.0), [], [onesB])
    masks = R2.alloc([4, 512], BF16)
    maskB = Buf("mask")
    DMA(S, "pool", masks, D["masks"].rearrange("p (a b) -> p a b", a=4), [], [maskB])

    hb = R1.alloc([8, SEQ], BF16)
    hbB = [bufs(NQB, f"hb{c}_") for c in range(8)]
    for tb in range(NQB):
        for c in range(8):
            DMA(S, "sp", hb[:, c, tb * 512:(tb + 1) * 512], D["h_blk"](c, tb), D["h_deps"](tb), [hbB[c][tb]])
    win, winB = load_w_bf16(C, R3, D["w_in"], 1024, 704, "win")
    cf = R3.alloc([4, 512], F32)
    cfB = bufs(4, "cf")
    sq = R3.alloc([4, 512], BF16)
    sqB = bufs(4, "sq")
    rs = R3.alloc([2, 512], F32)
    rsB = bufs(2, "rs")
    rt = R3.alloc([2, 512], F32)
    rtB = bufs(2, "rt")
    for tb in range(NQB):
        tsl = slice(tb * 512, (tb + 1) * 512)
        for oc in range(4):
            ps, psB = next_ps(C)
            for kc in range(8):
                MM(S, ps[:, :], win[:, kc, oc * 128:(oc + 1) * 128], hb[:, kc, tsl], kc == 0, kc == 7,
                   [winB[kc], hbB[kc][tb]], [psB])
            CP(S, "act", cf[:, oc, :], ps[:, :], [psB], [cfB[oc]])
            TT(S, "pool", sq[:, oc, :], cf[:, oc, :], cf[:, oc, :], ALU.mult, [cfB[oc]], [sqB[oc]])
        for lat in range(2):
            ps, psB = next_ps(C)
            for j in range(2):
                MM(S, ps[:, :], onesm[:, :], sq[:, 2 * lat + j, :], j == 0, j == 1, [onesB, sqB[2 * lat + j]], [psB])
            ACT(S, rs[:, lat, :], ps[:, :], AF.Sqrt, [psB], [rsB[lat]], bias=RMS_EPS)
            S.op("dve", (lambda o: (lambda e: e.reciprocal(o, o)))(rs[:, lat, :]), [rsB[lat]], [rsB[lat]])
            for j in range(2):
                oc = 2 * lat + j
                TT(S, "dve", cf[:, oc, :], cf[:, oc, :], rs[:, lat, :], ALU.mult, [cfB[oc], rsB[lat]], [cfB[oc]])
                dst, dB = (cqn, cqnB) if lat == 0 else (ckvn, ckvB)
                ACT(S, dst[:, j, tsl], cf[:, oc, :], AF.Identity, [cfB[oc], nrmB], [dB[j][tb]],
                    scale=nrm[:, oc:oc + 1])
        ps, psB = next_ps(C)
        ps2, ps2B = next_ps(C)
        for kc in range(8):
            MM(S, ps[0:96, :], win[:, kc, 512:608], hb[:, kc, tsl], kc == 0, kc == 7, [winB[kc], hbB[kc][tb]], [psB])
        for kc in range(8):
            MM(S, ps2[0:96, :], win[:, kc, 608:704], hb[:, kc, tsl], kc == 0, kc == 7, [winB[kc], hbB[kc][tb]], [ps2B])
        TT(S, "dve", rt[64:96, 0, :], ps[64:96, :], cosT[64:96, tsl], ALU.mult, [psB, tabB], [rtB[0]])
        TT(S, "dve", rt[64:96, 1, :], ps2[64:96, :], sinT[64:96, tsl], ALU.mult, [ps2B, tabB], [rtB[1]])
        TT(S, "pool", kpeT[64:96, tsl], rt[64:96, 0, :], rt[64:96, 1, :], ALU.add, [rtB[0], rtB[1]], [kpeB[tb]])
    S.barrier()
    R1.reset()
    R3.reset()

    wq, wqB = load_w_bf16(C, R3, D["wq"], 256, HPC * 96, "wq")
    wqs, wqsB = load_w_bf16(C, R3, D["wq_sw"], 256, HPC * 96, "wqs")
    wkk, wkkB = load_w_bf16(C, R3, D["wkv_k"], 256, HPC * 64, "wkk")
    wkv, wkvB = load_w_bf16(C, R3, D["wkv_v"], 256, HPC * 64, "wkv")
    kT = [R1.alloc([SEQ], BF16) for _ in range(2)]
    qT = [R1.alloc([SEQ], BF16) for _ in range(2)]
    V = [R1.alloc([32, 128], BF16) for _ in range(2)]
    kTB = [bufs(NQB, "kTa"), bufs(NQB, "kTb")]
    kT2B = bufs(2, "kTpe")
    qTB = [bufs(NQB, "qTa"), bufs(NQB, "qTb")]
    VB = [bufs(4, "Va"), bufs(4, "Vb")]
    V1B = bufs(2, "Vones")
    for i in range(2):
        S.op("pool", (lambda ap: (lambda e: e.memset(ap, 1.0)))(V[i][:, :, 64:128]), [], [V1B[i]])
    pT = [R3.alloc([512], BF16) for _ in range(4)]
    pTB = bufs(4, "pT")
    qr = R3.alloc([2, 512], F32)
    qrB = bufs(2, "qr")
    rden = [R3.alloc([512], F32) for _ in range(2)]
    rdB = bufs(2, "rden")
    ost = [R3.alloc([512], BF16) for _ in range(2)]
    ostB = bufs(2, "ost")
    outBs = []
    scale = float(96 ** -0.5)
    PS_S = (0, 1, 2, 5)
    PS_O = (3, 4)
    PS_X = (6, 7)
    xi = [0]

    def xps():
        i = PS_X[xi[0] % 2]
        xi[0] += 1
        return C.ps[i], C.psB[i]

    def prep_head(h):
        b = h % 2
        for tb in range(NQB):
            tsl = slice(tb * 512, (tb + 1) * 512)
            ps, psB = xps()
            for kc in range(2):
                MM(S, ps[0:64, :], wkk[:, kc, h * 64:(h + 1) * 64], ckvn[:, kc, tsl], kc == 0, kc == 1,
                   [wkkB[kc], ckvB[kc][tb]], [psB])
            CP(S, "act", kT[b][0:64, tsl], ps[0:64, :], [psB], [kTB[b][tb]])
        CP(S, "pool", kT[b][64:96, :], kpeT[64:96, :], kpeB, [kT2B[b]])
        for g in range(4):
            ps, psB = xps()
            for j in range(8):
                kt = g * 8 + j
                for kc in range(2):
                    MM(S, ps[:, j * 64:(j + 1) * 64], ckvn[:, kc, kt * 128:(kt + 1) * 128],
                       wkv[:, kc, h * 64:(h + 1) * 64], kc == 0, kc == 1,
                       [wkvB[kc], ckvB[kc][kt // 4]], [psB])
            CP(S, "dve", V[b][:, g * 8:(g + 1) * 8, 0:64], ps[:, :].rearrange("p (a b) -> p a b", a=8),
               [psB], [VB[b][g]])
        for tb in range(NQB):
            tsl = slice(tb * 512, (tb + 1) * 512)
            ps, psB = xps()
            ps2, ps2B = xps()
            for kc in range(2):
                MM(S, ps[0:96, :], wq[:, kc, h * 96:(h + 1) * 96], cqn[:, kc, tsl], kc == 0, kc == 1,
                   [wqB[kc], cqnB[kc][tb]], [psB])
            for kc in range(2):
                MM(S, ps2[0:96, :], wqs[:, kc, h * 96:(h + 1) * 96], cqn[:, kc, tsl], kc == 0, kc == 1,
                   [wqsB[kc], cqnB[kc][tb]], [ps2B])
            TS(S, "dve", qT[b][0:64, tsl], ps[0:64, :], scale, None, ALU.mult, None, [psB], [qTB[b][tb]])
            STT(S, "dve", qr[64:96, 0, :], ps[64:96, :], scale, cosT[64:96, tsl], ALU.mult, ALU.mult,
                [psB, tabB], [qrB[0]])
            STT(S, "dve", qr[64:96, 1, :], ps2[64:96, :], scale, sinT[64:96, tsl], ALU.mult, ALU.mult,
                [ps2B, tabB], [qrB[1]])
            TT(S, "pool", qT[b][64:96, tsl], qr[64:96, 0, :], qr[64:96, 1, :], ALU.add, [qrB[0], qrB[1]],
               [qTB[b][tb]])

    DEPTH = 3
    for h in range(HPC):
        prep_head(h)
        b = h % 2
        pairs = [(qb, kt) for qb in range(NQB) for kt in range(4 * qb + 4)]

        def emit_qk(i, b=b):
            qb, kt = pairs[i]
            i4 = i % 4
            qsl = slice(qb * 512, (qb + 1) * 512)
            s_ps, s_psB = C.ps[PS_S[i4]], C.psB[PS_S[i4]]
            MM(S, s_ps[:, :], kT[b][0:96, kt * 128:(kt + 1) * 128], qT[b][0:96, qsl], True, True,
               [kTB[b][kt // 4], kT2B[b], qTB[b][qb]], [s_psB])
            ACT(S, pT[i4], s_ps[:, :], AF.Exp, [s_psB], [pTB[i4]])
            if kt >= 4 * qb:
                TT(S, "pool", pT[i4], pT[i4], masks[:, kt - 4 * qb, :], ALU.mult, [pTB[i4], maskB], [pTB[i4]])

        def emit_pv(i, b=b, h=h):
            qb, kt = pairs[i]
            i4 = i % 4
            qsl = slice(qb * 512, (qb + 1) * 512)
            nkt = 4 * qb + 4
            po = PS_O[qb % 2]
            o_ps, o_psB = C.ps[po], C.psB[po]
            MM(S, o_ps[:, :], V[b][:, kt, :], pT[i4], kt == 0, kt == nkt - 1,
               [VB[b][kt // 8], V1B[b], pTB[i4]], [o_psB])
            if kt == nkt - 1:
                r2 = qb % 2
                S.op("dve", (lambda o, i_: (lambda e: e.reciprocal(o, i_)))(rden[r2][64:128, :], o_ps[64:128, :]),
                     [o_psB], [rdB[r2]])
                TT(S, "dve", ost[r2][0:64, :], o_ps[0:64, :], rden[r2][64:128, :], ALU.mult, [o_psB, rdB[r2]], [ostB[r2]])
                ob = Buf("out")
                outBs.append(ob)
                DMA(S, "sp", D["o_blk"](h, qb), ost[r2][0:64, :], [ostB[r2]], [ob])

        n_before = len(outBs)
        for i in range(len(pairs) + DEPTH):
            if i < len(pairs):
                emit_qk(i)
            if i >= DEPTH:
                emit_pv(i - DEPTH)
        if "after_head" in D:
            D["after_head"](h, outBs[n_before:])
    return outBs


def consts_O():
    half = 16
    inv = (10000.0 ** (-np.arange(half, dtype=np.float32) / half)).astype(np.float32)
    ang = np.arange(SEQ, dtype=np.float32)[:, None] * inv[None, :]
    cos, sin = np.cos(ang).astype(np.float32), np.sin(ang).astype(np.float32)
    cosT = np.zeros((96, SEQ), np.float32)
    sinT = np.zeros((96, SEQ), np.float32)
    cosT[64:80] = cos.T; cosT[80:96] = cos.T
    sinT[64:80] = -sin.T; sinT[80:96] = sin.T
    p = np.arange(128)[:, None]
    f = np.arange(512)[None, :]
    masks = np.concatenate([(f >= p + 128 * d).astype(np.float32) for d in range(4)], axis=1)
    return {"cosT": cosT, "sinT": sinT, "masks": np.ascontiguousarray(masks)}


def prep_O_weights(w_in, q_norm, kv_norm, wq_b, wkv_b, hs):
    pad = np.zeros((1024, 64), np.float32)
    kpe = w_in[:, 512:544]
    kpe_sw = np.concatenate([kpe[:, 16:32], kpe[:, 0:16]], axis=1)
    w_in_ext = np.ascontiguousarray(np.concatenate([w_in[:, 0:512], pad, kpe, pad, kpe_sw], axis=1))
    nrm = np.ascontiguousarray(np.concatenate([q_norm.reshape(2, 128).T, kv_norm.reshape(2, 128).T], axis=1))
    heads = range(hs * HPC, (hs + 1) * HPC)
    wq = np.concatenate([wq_b[:, h * 96:(h + 1) * 96] for h in heads], axis=1)
    wq_sw = np.concatenate([np.concatenate([wq_b[:, h * 96:h * 96 + 64], wq_b[:, h * 96 + 80:h * 96 + 96],
                                            wq_b[:, h * 96 + 64:h * 96 + 80]], axis=1) for h in heads], axis=1)
    wkv_k = np.concatenate([wkv_b[:, h * 128:h * 128 + 64] for h in heads], axis=1)
    wkv_v = np.concatenate([wkv_b[:, h * 128 + 64:(h + 1) * 128] for h in heads], axis=1)
    return dict(w_in=w_in_ext, nrm=nrm.astype(np.float32), wq=np.ascontiguousarray(wq), wq_sw=np.ascontiguousarray(wq_sw),
                wkv_k=np.ascontiguousarray(wkv_k), wkv_v=np.ascontiguousarray(wkv_v))


TBR = 256
NBR = SEQ // TBR
CH = 128
GELU_C = 1.5957691216057308
NEG_EHALF = -0.6065306597126334
GN_EPS = 64e-5


def gelu_ops(S, dst, x, tmp1, tmp2, xB, t1B, t2B, dstB):
    TT(S, "pool", tmp1, x, x, ALU.mult, [xB], [t1B])
    TS(S, "dve", tmp1, tmp1, 0.044715, 1.0, ALU.mult, ALU.add, [t1B], [t1B])
    TT(S, "pool", tmp1, tmp1, x, ALU.mult, [t1B, xB], [t1B])
    ACT(S, tmp2, tmp1, AF.Sigmoid, [t1B], [t2B], scale=GELU_C)
    TT(S, "dve", dst, x, tmp2, ALU.mult, [xB, t2B], [dstB])


def stage_E(C, D):
    S = C.S
    KB = 1024
    R1 = Region(C.arena, 0, 64 * KB)
    R2 = Region(C.arena, 64 * KB, C.arena_bytes - 64 * KB)
    hb = R1.alloc([8, SEQ], BF16)
    hbB = [bufs(SEQ // 512, f"hb{c}_") for c in range(8)]
    for tb in range(SEQ // 512):
        for c in range(8):
            DMA(S, "sp", hb[:, c, tb * 512:(tb + 1) * 512], D["h_blk"](c, tb), D["h_deps"](tb), [hbB[c][tb]])
    ident = R2.alloc([4, 128], F32)
    cB = Buf("consts")
    for i in range(4):
        DMA(S, "sp", ident[:, i, :], D["ident"], [], [cB])
    mLQ = R2.alloc([2, 256], F32)
    for i in range(2):
        DMA(S, "sp", mLQ[:, i, :], D["maskLQ"], [], [cB])
    mSL = R2.alloc([4, 128], F32)
    for i in range(4):
        DMA(S, "sp", mSL[:, i, :], D["maskSL"], [], [cB])
    blk = R2.alloc([128], F32)
    DMA(S, "sp", blk, D["blk"], [], [cB])
    blk64 = R2.alloc([128], F32)
    S.op("dve", lambda e: e.tensor_scalar(blk64, blk, 1.0 / 64.0, None, ALU.mult), [cB], [cB])
    ones = R2.alloc([128], F32)
    S.op("pool", lambda e: e.memset(ones, 1.0), [], [cB])
    c_mark = R2.off

    hsg = R2.alloc([8, 2048], BF16)
    hsgB = [bufs(4, f"hsg{c}_") for c in range(8)]
    for tb in range(4):
        for c in range(8):
            DMA(S, "sp", hsg[:, c, tb * 512:(tb + 1) * 512], D["hsg_blk"](c, tb), [], [hsgB[c][tb]])
    wpu, wpuB = load_w_bf16(C, R2, D["w_pu"], 1024, 512, "wpu")
    wpv, wpvB = load_w_bf16(C, R2, D["w_pv"], 1024, 512, "wpv")
    wsf = R2.alloc([4, 128], F32)
    wsB = Buf("ws")
    DMA(S, "sp", wsf, D["wsT"].rearrange("g s t -> s g t"), [], [wsB])
    wsm = R2.alloc([4, 128], BF16)
    for g in range(4):
        TT(S, "dve", wsm[:, g, :], wsf[:, g, :], mLQ[:, 0, 128:256], ALU.mult, [wsB, cB], [wsB])
    bsb = R2.alloc([4, 128], F32)
    lng = R2.alloc([512], F32)
    lnb = R2.alloc([512], F32)
    sgB = Buf("sgc")
    DMA(S, "sp", bsb, D["sg_b"].partition_broadcast(128).rearrange("p (g t) -> p g t", g=4), [], [sgB])
    DMA(S, "sp", lng, D["sg_lng"].partition_broadcast(128), [], [sgB])
    DMA(S, "sp", lnb, D["sg_lnb"].partition_broadcast(128), [], [sgB])
    xv = R2.alloc([512], F32); t1 = R2.alloc([512], F32); t2 = R2.alloc([512], F32); zg = R2.alloc([512], F32)
    xu = R2.alloc([512], F32); u1 = R2.alloc([512], F32); u2 = R2.alloc([512], F32); ug = R2.alloc([512], F32)
    zb = R2.alloc([512], BF16)
    st6 = R2.alloc([4, 6], F32); mv = R2.alloc([4, 2], F32); rstd4 = R2.alloc([4], F32)
    osg = [R2.alloc([4, 128], F32) for _ in range(2)]
    osg16 = [R2.alloc([4, 128], BF16) for _ in range(2)]
    osg16B = bufs(2, "osg16")
    xvB, t1B, t2B, zgB, xuB, u1B, u2B, ugB, zbB, stB = (Buf(n) for n in "xv t1 t2 zg xu u1 u2 ug zb st".split())
    osgB = bufs(2, "osg")
    outBs = []
    for ci in range(16):
        tok0 = ci * 128
        tsl = slice(tok0, tok0 + 128)
        tb = tok0 // 512
        ps, psB = next_ps(C)
        for kc in range(8):
            MM(S, ps[:, :], hsg[:, kc, tsl], wpv[:, kc, :], kc == 0, kc == 7, [hsgB[kc][tb], wpvB[kc]], [psB])
        CP(S, "act", xv, ps[:, :], [psB], [xvB])
        gelu_ops(S, zg, xv, t1, t2, xvB, t1B, t2B, zgB)
        for g in range(4):
            S.op("dve", (lambda o, i: (lambda e: e.bn_stats(o, i)))(st6[:, g, :], zg[:, g * 128:(g + 1) * 128]), [zgB], [stB])
            S.op("dve", (lambda o, i: (lambda e: e.bn_aggr(o, i)))(mv[:, g, :], st6[:, g, :]), [stB], [stB])
        ACT(S, rstd4, mv[:, :, 1], AF.Sqrt, [stB], [stB], bias=LN_EPS)
        S.op("dve", lambda e: e.reciprocal(rstd4, rstd4), [stB], [stB])
        for g in range(4):
            TS(S, "dve", zg[:, g * 128:(g + 1) * 128], zg[:, g * 128:(g + 1) * 128], mv[:, g, 0:1], rstd4[:, g:g + 1],
               ALU.subtract, ALU.mult, [zgB, stB], [zgB])
        TT(S, "pool", zg, zg, lng, ALU.mult, [zgB, sgB], [zgB])
        TT(S, "pool", zb, zg, lnb, ALU.add, [zgB, sgB], [zbB])
        pu, puB = next_ps(C)
        for g in range(4):
            for kc in range(8):
                MM(S, pu[:, g * 128:(g + 1) * 128], wpu[:, kc, g * 128:(g + 1) * 128], hsg[:, kc, tsl], kc == 0, kc == 7,
                   [hsgB[kc][tb], wpuB[kc]], [puB])
        CP(S, "act", xu, pu[:, :], [puB], [xuB])
        gelu_ops(S, ug, xu, u1, u2, xuB, u1B, u2B, ugB)
        pz, pzB = next_ps(C)
        for g in range(4):
            MM(S, pz[:, g * 128:(g + 1) * 128], zb[:, g * 128:(g + 1) * 128], wsm[:, g, :], True, True, [zbB, wsB], [pzB])
        o_ = osg[ci % 2]
        oB_ = osgB[ci % 2]
        TT(S, "dve", o_, pz[:, :].rearrange("p (g t) -> p g t", g=4), bsb, ALU.add, [pzB, sgB], [oB_])
        TT(S, "pool", osg16[ci % 2], o_, ug.rearrange("p (g t) -> p g t", g=4), ALU.mult, [oB_, ugB], [osg16B[ci % 2]])
        ob = Buf("out")
        outBs.append(ob)
        DMA(S, "sp", D["yb_blk"](ci), osg16[ci % 2], [osg16B[ci % 2]], [ob])
    S.barrier()
    R2.off = c_mark

    wrw, wrwB = load_w_bf16(C, R2, D["w_rw"], 1024, 1024, "wrw")
    mu = R2.alloc([8], F32)
    vec = R2.alloc([2, 8], F32)
    w2a2 = R2.alloc([256], F32)
    g2 = R2.alloc([256], F32)
    pB = Buf("rwp")
    DMA(S, "sp", mu, D["mu"], [], [pB])
    DMA(S, "sp", vec[:, :, 0:7], D["vec"], [], [pB])
    DMA(S, "sp", w2a2, D["w2a2"], [], [pB])
    DMA(S, "sp", g2, D["g2"], [], [pB])
    for j in range(2):
        TS(S, "dve", vec[:, j, 7:8], vec[:, j, 3:4], -1.0, 1.0, ALU.mult, ALU.add, [pB], [pB])
    V_W0, V_A0, V_KK, V_KA, V_RK, V_GG, V_GB, V_OM = range(8)

    def vv_(j, i):
        return vec[:, j, i:i + 1]

    pbuf = R2.alloc([8, TBR + 1], F32)
    pbB = bufs(8, "pbuf")
    for oc in range(8):
        S.op("pool", (lambda ap: (lambda e: e.memset(ap, 0.0)))(pbuf[:, oc, 0:1]), [], [pbB[oc]])
    xs = R2.alloc([8, TBR], F32)
    xsB = bufs(8, "xs")
    dtmp = R2.alloc([2, TBR], F32)
    dtB = bufs(2, "dtmp")
    tw = R2.alloc([TBR], F32); sgl = R2.alloc([TBR], F32)
    twB, sglB = Buf("tw"), Buf("sgl")
    NTMP = 12
    tmp = [R2.alloc([TBR], F32) for _ in range(NTMP)]
    tmpB = bufs(NTMP, "tmp")
    tmp2 = [R2.alloc([TBR], F32) for _ in range(NTMP)]
    tmp2B = bufs(NTMP, "tmp2")
    gbuf = R2.alloc([2, TBR], F32); gbB = bufs(2, "g")
    bonus = R2.alloc([2, TBR], F32); boB = bufs(2, "bonus")
    KR = R2.alloc([2 * 2 * 2, 128], F32)
    KRB = [[Buf(f"KR{j}{c}") for c in range(2)] for j in range(2)]
    Kh = R2.alloc([2, TBR], F32); KhB = bufs(2, "Kh")
    Bh = R2.alloc([2, TBR], F32); BhB = bufs(2, "Bh")
    Kb = R2.alloc([2, TBR], F32); KbB = bufs(2, "Kb")
    Bb = R2.alloc([2, TBR], F32); BbB = bufs(2, "Bb")
    gC = R2.alloc([2, 2], F32); gCB = [[Buf(f"gC{j}{c}") for c in range(2)] for j in range(2)]
    TM = R2.alloc([2 * 2 * 4, 128], F32)
    TMB = [[Buf(f"TM{j}{c}") for c in range(2)] for j in range(2)]
    TK16 = R2.alloc([2 * 2, 128], BF16)
    TK16B = [[Buf(f"TK{j}{c}") for c in range(2)] for j in range(2)]
    cbs = []
    for _c in range(2):
        cbs.append(dict(
            LQk=R2.alloc([4, 256], F32), LQkB=bufs(2, "LQk"), LQb=R2.alloc([4, 256], F32), LQbB=bufs(2, "LQb"),
            Nb=[R2.alloc([4, 128], BF16) for _ in range(2)], NbB=bufs(2, "N"),
            Mb=[R2.alloc([4, 128], BF16) for _ in range(2)], MbB=bufs(2, "M"),
            Pb=R2.alloc([4, 128], BF16), PbB=Buf("P"), Xs=R2.alloc([4, 64], BF16), XsB=Buf("X"),
            Ut=R2.alloc([4, 64], F32), UtB=Buf("Ut"), Wt=R2.alloc([2, 128], F32), WtB=Buf("Wt"),
            Un=R2.alloc([4, 64], F32), UnB=Buf("Un")))
    Ast = [R2.alloc([2, 64], F32) for _ in range(2)]
    AB = [[Buf("A0e"), Buf("A0o")], [Buf("A1e"), Buf("A1o")]]
    S.op("pool", lambda e: e.memset(Ast[0], 0.0), [], [AB[0][0], AB[0][1]])
    yT = R2.alloc([2, TBR], F32); yTB = [[Buf("yTe0"), Buf("yTe1")], [Buf("yTo0"), Buf("yTo1")]]
    yo = [R2.alloc([TBR], BF16) for _ in range(2)]; yoB = bufs(2, "yo")
    PB_A = (0, 1)
    PB_Y = (2, 3)
    rot = [4, 5, 6, 7]
    ri = [0]

    def rps():
        i = rot[ri[0] % 4]
        ri[0] += 1
        return C.ps[i], C.psB[i]

    cur = 0
    for blk_i in range(NBR):
        t0 = blk_i * TBR
        tsl = slice(t0, t0 + TBR)
        tb = t0 // 512
        for oc in range(8):
            ps, psB = rps()
            for kc in range(8):
                MM(S, ps[:, 0:TBR], wrw[:, kc, oc * 128:(oc + 1) * 128], hb[:, kc, tsl], kc == 0, kc == 7,
                   [wrwB[kc], hbB[kc][tb]], [psB])
            CP(S, "act", pbuf[:, oc, 1:TBR + 1], ps[:, 0:TBR], [psB], [pbB[oc]])
            k2 = oc % 2
            TT(S, "dve", dtmp[:, k2, :], pbuf[:, oc, 0:TBR], pbuf[:, oc, 1:TBR + 1], ALU.subtract, [pbB[oc]], [dtB[k2]])
            STT(S, "dve", xs[:, oc, :], dtmp[:, k2, :], mu[:, oc:oc + 1], pbuf[:, oc, 1:TBR + 1], ALU.mult, ALU.add,
                [dtB[k2], pbB[oc], pB], [xsB[oc]])
            CP(S, "act", pbuf[:, oc, 0:1], pbuf[:, oc, TBR:TBR + 1], [pbB[oc]], [pbB[oc]])
        ACT(S, tw[0:64, :], xs[0:64, 6, :], AF.Tanh, [xsB[6]], [twB])
        ACT(S, sgl, xs[:, 7, :], AF.Sigmoid, [xsB[7]], [sglB])
        def prel_gen(j):
            r_, k_, v_ = xs[:, j, :], xs[:, 2 + j, :], xs[:, 4 + j, :]
            rB_, kB_, vB_ = xsB[j], xsB[2 + j], xsB[4 + j]
            T = lambda i: (tmp if j == 0 else tmp2)[i]
            TBf = lambda i: (tmpB if j == 0 else tmp2B)[i]
            yield
            ps, psB = rps()
            yield
            MM(S, ps[:, 0:TBR], w2a2[0:64, j * 128:(j + 1) * 128], tw[0:64, :], True, True, [pB, twB], [psB])
            yield
            ACT(S, T(0), ps[:, 0:TBR], AF.Sigmoid, [psB, pB], [TBf(0)], bias=vv_(j, V_W0))
            yield
            TS(S, "dve", T(0), T(0), NEG_EHALF, None, ALU.mult, None, [TBf(0)], [TBf(0)])
            yield
            ps, psB = rps()
            yield
            MM(S, ps[:, 0:TBR], w2a2[64:128, j * 128:(j + 1) * 128], xs[64:128, 6, :], True, True, [pB, xsB[6]], [psB])
            yield
            ACT(S, T(1), ps[:, 0:TBR], AF.Sigmoid, [psB, pB], [TBf(1)], bias=vv_(j, V_A0))
            yield
            ps, psB = rps()
            yield
            MM(S, ps[:, 0:TBR], g2[:, j * 128:(j + 1) * 128], sgl, True, True, [pB, sglB], [psB])
            yield
            CP(S, "act", gbuf[:, j, :], ps[:, 0:TBR], [psB], [gbB[j]])
            yield
            TS(S, "dve", T(2), k_, vv_(j, V_KK), None, ALU.mult, None, [kB_, pB], [TBf(2)])
            yield
            TT(S, "pool", T(3), T(2), T(2), ALU.mult, [TBf(2)], [TBf(3)])
            yield
            ps, psB = rps()
            yield
            MM(S, ps[:, 0:TBR], blk[:, :], T(3), True, True, [cB, TBf(3)], [psB])
            yield
            ACT(S, T(3), ps[:, 0:TBR], AF.Sqrt, [psB], [TBf(3)])
            yield
            TS(S, "dve", T(3), T(3), 1e-12, None, ALU.max, None, [TBf(3)], [TBf(3)])
            yield
            S.op("dve", (lambda o: (lambda e: e.reciprocal(o, o)))(T(3)), [TBf(3)], [TBf(3)])
            yield
            TT(S, "pool", T(2), T(2), T(3), ALU.mult, [TBf(2), TBf(3)], [TBf(2)])
            yield
            TS(S, "dve", T(4), T(1), vv_(j, V_KA), vv_(j, V_OM), ALU.mult, ALU.add, [TBf(1), pB], [TBf(4)])
            yield
            TT(S, "pool", T(4), T(4), k_, ALU.mult, [TBf(4), kB_], [TBf(4)])
            yield
            TT(S, "pool", T(5), T(2), T(1), ALU.mult, [TBf(2), TBf(1)], [TBf(5)])
            yield
            STT(S, "dve", T(6), r_, vv_(j, V_RK), T(4), ALU.mult, ALU.mult, [rB_, pB, TBf(4)], [TBf(6)])
            yield
            ps, psB = rps()
            yield
            MM(S, ps[:, 0:TBR], blk[:, :], T(6), True, True, [cB, TBf(6)], [psB])
            yield
            TT(S, "dve", bonus[:, j, :], ps[:, 0:TBR], v_, ALU.mult, [psB, vB_], [boB[j]])
            yield
            for c in range(2):
                csl = slice(c * CH, (c + 1) * CH)
                S.op("dve", (lambda o, d1: (lambda e: e.tensor_tensor_scan(o, ones[:, :], d1, 0.0, ALU.mult, ALU.add)))(
                    T(7)[:, csl], T(0)[:, csl]), [TBf(0), cB], [TBf(7)])
            yield
            TT(S, "pool", T(8), T(7), T(0), ALU.subtract, [TBf(7), TBf(0)], [TBf(8)])
            yield
            ACT(S, T(9), T(7), AF.Exp, [TBf(7)], [TBf(9)])
            yield
            ACT(S, T(8), T(8), AF.Exp, [TBf(8)], [TBf(8)])
            yield
            ACT(S, T(10), T(7), AF.Exp, [TBf(7)], [TBf(10)], scale=-1.0)
            yield
            for c in range(2):
                csl = slice(c * CH, (c + 1) * CH)
                last = T(7)[:, (c + 1) * CH - 1:(c + 1) * CH]
                ACT(S, T(11)[:, csl], T(7)[:, csl], AF.Exp, [TBf(7)], [TBf(11)], scale=-1.0, bias=last)
                CP(S, "dve", gC[:, j, c:c + 1], T(9)[:, (c + 1) * CH - 1:(c + 1) * CH], [TBf(9)], [gCB[j][c]])
            krv = KR.rearrange("p (j c w) t -> p j c w t", j=2, c=2)
            yield
            TT(S, "dve", krv[:, j, :, 1, :], r_.rearrange("p (c t) -> p c t", c=2), T(9).rearrange("p (c t) -> p c t", c=2),
               ALU.mult, [rB_, TBf(9)], [KRB[j][0], KRB[j][1]])
            yield
            TT(S, "pool", krv[:, j, :, 0, :], T(2).rearrange("p (c t) -> p c t", c=2), T(8).rearrange("p (c t) -> p c t", c=2),
               ALU.mult, [TBf(2), TBf(8)], [KRB[j][0], KRB[j][1]])
            yield
            TT(S, "dve", Kh[:, j, :], T(4), T(10), ALU.mult, [TBf(4), TBf(10)], [KhB[j]])
            yield
            TT(S, "pool", Bh[:, j, :], T(5), T(10), ALU.mult, [TBf(5), TBf(10)], [BhB[j]])
            yield
            TT(S, "dve", Kb[:, j, :], T(4), T(11), ALU.mult, [TBf(4), TBf(11)], [KbB[j]])
            yield
            TT(S, "pool", Bb[:, j, :], T(5), T(11), ALU.mult, [TBf(5), TBf(11)], [BbB[j]])
            tmv = TM.rearrange("p (j c a) f -> p j c a f", j=2, c=2)
            yield
            for c in range(2):
                csl = slice(c * CH, (c + 1) * CH)
                ps, psB = rps()
                TR(S, ps[:, 0:128], v_[:, csl], ident[:, 0, :], [vB_, cB], [psB])
                TR(S, ps[:, 128:256], krv[:, j, c, 0, :], ident[:, 0, :], [KRB[j][c], cB], [psB])
                TR(S, ps[:, 256:384], Kb[:, j, csl], ident[:, 0, :], [KbB[j], cB], [psB])
                TR(S, ps[:, 384:512], Bb[:, j, csl], ident[:, 0, :], [BbB[j], cB], [psB])
                CP(S, "act", tmv[:, j, c, :, :], ps[:, :].rearrange("p (a f) -> p a f", a=4), [psB], [TMB[j][c]])
                CP(S, "act", TK16[:, j * 2 + c, :], ps[:, 128:256], [psB], [TK16B[j][c]])
        gens = [prel_gen(0), prel_gen(1)]
        while gens:
            for g_ in list(gens):
                try:
                    next(g_)
                except StopIteration:
                    gens.remove(g_)
        krv = KR.rearrange("p (j c w) t -> p j c w t", j=2, c=2)
        tmv = TM.rearrange("p (j c a) f -> p j c a f", j=2, c=2)
        def e3_gen(c):
            csl = slice(c * CH, (c + 1) * CH)
            cb = cbs[c]
            LQk, LQkB, LQb, LQbB, Nb, NbB, Mb, MbB = (cb[k] for k in "LQk LQkB LQb LQbB Nb NbB Mb MbB".split())
            Pb, PbB, Xs, XsB, Ut, UtB, Wt, WtB = (cb[k] for k in "Pb PbB Xs XsB Ut UtB Wt WtB".split())
            lqkv = LQk.rearrange("p (j hh) w -> p hh j w", hh=2)
            lqbv = LQb.rearrange("p (j hh) w -> p hh j w", hh=2)
            nbv = Nb[0].rearrange("p (j hh) w -> p hh j w", hh=2)
            for hh in range(2):
                base = 64 * hh
                pa, paB = rps()
                pb_, pbB_ = rps()
                pc, pcB = rps()
                for j in range(2):
                    rhs = krv[base:base + 64, j, c, :, :].rearrange("p w t -> p (w t)")
                    MM(S, pa[:, j * 256:(j + 1) * 256], Kh[base:base + 64, j, csl], rhs, True, True,
                       [KhB[j], KRB[j][c]], [paB])
                    MM(S, pb_[:, j * 256:(j + 1) * 256], Bh[base:base + 64, j, csl], rhs, True, True,
                       [BhB[j], KRB[j][c]], [pbB_])
                    MM(S, pc[:, j * 128:(j + 1) * 128], krv[base:base + 64, j, c, 0, :], Bh[base:base + 64, j, csl],
                       True, True, [KRB[j][c], BhB[j]], [pcB])
                TT(S, "dve", lqkv[:, hh, :, :], pa[:, :].rearrange("p (a b) -> p a b", a=2), mLQ, ALU.mult,
                   [paB, cB], [LQkB[hh]])
                TT(S, "dve", lqbv[:, hh, :, :], pb_[:, :].rearrange("p (a b) -> p a b", a=2), mLQ, ALU.mult,
                   [pbB_, cB], [LQbB[hh]])
                STT(S, "dve", nbv[:, hh, :, :], pc[:, 0:256].rearrange("p (a b) -> p a b", a=2), -1.0, mSL[:, 0:2, :],
                    ALU.mult, ALU.mult, [pcB, cB], [NbB[0]])
            yield
            n0, m0 = 0, 0
            TS(S, "pool", Mb[0], LQb[:, :, 0:128], -1.0, None, ALU.mult, None, [LQbB[0], LQbB[1]], [MbB[0]])
            TT(S, "pool", Pb, Mb[0], ident, ALU.add, [MbB[0], cB], [PbB])
            for lvl in range(1, 7):
                n1, m1 = 1 - n0, 1 - m0
                pn, pnB = rps()
                for h in range(4):
                    MM(S, pn[:, h * 128:(h + 1) * 128], Mb[m0][:, h, :], Nb[n0][:, h, :], True, True,
                       [MbB[m0], NbB[n0]], [pnB])
                if lvl <= 5:
                    pm, pmB = rps()
                    for h in range(4):
                        MM(S, pm[:, h * 128:(h + 1) * 128], Nb[n0][:, h, :], Mb[m0][:, h, :], True, True,
                           [MbB[m0], NbB[n0]], [pmB])
                yield
                CP(S, "act", Nb[n1], pn[:, :].rearrange("p (a b) -> p a b", a=4), [pnB], [NbB[n1]])
                if lvl <= 5:
                    CP(S, "dve", Mb[m1], pm[:, :].rearrange("p (a b) -> p a b", a=4), [pmB], [MbB[m1]])
                yield
                pp, ppB = rps()
                for h in range(4):
                    MM(S, pp[:, h * 128:(h + 1) * 128], Nb[n1][:, h, :], Pb[:, h, :], True, True, [NbB[n1], PbB], [ppB])
                yield
                TT(S, "dve", Pb, Pb, pp[:, :].rearrange("p (a b) -> p a b", a=4), ALU.add, [PbB, ppB], [PbB])
                n0, m0 = n1, m1
            px, pxB = rps()
            for h in range(4):
                j, base = h // 2, 64 * (h % 2)
                MM(S, px[:, h * 64:(h + 1) * 64], LQk[:, h, 0:128], tmv[:, j, c, 0, base:base + 64], True, True,
                   [LQkB[h % 2], TMB[j][c]], [pxB])
            yield
            CP(S, "act", Xs, px[:, 0:256].rearrange("p (a b) -> p a b", a=4), [pxB], [XsB])
            yield
            pu_, puB_ = rps()
            for h in range(4):
                MM(S, pu_[:, h * 64:(h + 1) * 64], Pb[:, h, :], Xs[:, h, :], True, True, [PbB, XsB], [puB_])
            CP(S, "dve", Ut, pu_[:, 0:256].rearrange("p (a b) -> p a b", a=4), [puB_], [UtB])
            pw, pwB = rps()
            pw2, pw2B = rps()
            for h in range(4):
                j, hh = h // 2, h % 2
                base = 64 * hh
                tgt, tgtB = (pw, pwB) if hh == 0 else (pw2, pw2B)
                MM(S, tgt[base:base + 64, j * 128:(j + 1) * 128], TK16[:, j * 2 + c, base:base + 64], Pb[:, h, :], True, True,
                   [TK16B[j][c], PbB], [tgtB])
            CP(S, "act", Wt[0:64, :, :], pw[0:64, 0:256].rearrange("p (a b) -> p a b", a=2), [pwB], [WtB])
            CP(S, "act", Wt[64:128, :, :], pw2[64:128, 0:256].rearrange("p (a b) -> p a b", a=2), [pw2B], [WtB])

        gens = [e3_gen(0), e3_gen(1)]
        while gens:
            for g_ in list(gens):
                try:
                    next(g_)
                except StopIteration:
                    gens.remove(g_)
        for c in range(2):
            csl = slice(c * CH, (c + 1) * CH)
            cb = cbs[c]
            LQk, LQkB, LQb, LQbB = cb["LQk"], cb["LQkB"], cb["LQb"], cb["LQbB"]
            Ut, UtB, Wt, WtB, Un, UnB = cb["Ut"], cb["UtB"], cb["Wt"], cb["WtB"], cb["Un"], cb["UnB"]
            A0, A1 = Ast[cur], Ast[1 - cur]
            A0B, A1B = AB[cur], AB[1 - cur]
            for h in range(4):
                j, hh = h // 2, h % 2
                base = 64 * hh
                pA, pAB = C.ps[PB_A[hh]], C.psB[PB_A[hh]]
                MM(S, pA[:, j * 64:(j + 1) * 64], Wt[base:base + 64, j, :], A0[base:base + 64, j, :], True, True,
                   [WtB, A0B[hh]], [pAB])
            for hh in range(2):
                pA, pAB = C.ps[PB_A[hh]], C.psB[PB_A[hh]]
                utv = Ut.rearrange("p (j hh) v -> p hh j v", hh=2)
                unv = Un.rearrange("p (j hh) v -> p hh j v", hh=2)
                STT(S, "dve", unv[:, hh, :, :], pA[:, 0:128].rearrange("p (j v) -> p j v", j=2), -1.0, utv[:, hh, :, :],
                    ALU.mult, ALU.subtract, [pAB, UtB], [UnB])
            for h in range(4):
                j, hh = h // 2, h % 2
                base = 64 * hh
                pY, pYB = C.ps[PB_Y[hh]], C.psB[PB_Y[hh]]
                osl = pY[base:base + 64, j * 128:(j + 1) * 128]
                MM(S, osl, A0[base:base + 64, j, :], krv[base:base + 64, j, c, 1, :], True, False, [A0B[hh], KRB[j][c]], [pYB])
                MM(S, osl, tmv[:, j, c, 0, base:base + 64], LQk[:, h, 128:256], False, False, [TMB[j][c], LQkB[hh]], [pYB])
                MM(S, osl, Un[:, h, :], LQb[:, h, 128:256], False, True, [UnB, LQbB[hh]], [pYB])
            for h in range(4):
                j, hh = h // 2, h % 2
                base = 64 * hh
                pA, pAB = C.ps[PB_A[hh]], C.psB[PB_A[hh]]
                osl = pA[base:base + 64, 128 + j * 64:128 + (j + 1) * 64]
                MM(S, osl, tmv[:, j, c, 2, base:base + 64], tmv[:, j, c, 0, base:base + 64], True, False, [TMB[j][c]], [pAB])
                MM(S, osl, tmv[:, j, c, 3, base:base + 64], Un[:, h, :], False, True, [TMB[j][c], UnB], [pAB])
            for h in range(4):
                j, hh = h // 2, h % 2
                base = 64 * hh
                pA, pAB = C.ps[PB_A[hh]], C.psB[PB_A[hh]]
                STT(S, "dve", A1[base:base + 64, j, :], A0[base:base + 64, j, :], gC[base:base + 64, j, c:c + 1],
                    pA[base:base + 64, 128 + j * 64:128 + (j + 1) * 64], ALU.mult, ALU.add,
                    [A0B[hh], gCB[j][c], pAB], [A1B[hh]])
            for hh in range(2):
                base = 64 * hh
                pY, pYB = C.ps[PB_Y[hh]], C.psB[PB_Y[hh]]
                CP(S, "act", yT[base:base + 64, :, csl], pY[base:base + 64, 0:256].rearrange("p (j t) -> p j t", j=2),
                   [pYB], [yTB[hh][c]])
            cur = 1 - cur
        for j in range(2):
            yB_all = [yTB[0][0], yTB[0][1], yTB[1][0], yTB[1][1]]
            ps, psB = rps()
            MM(S, ps[:, 0:TBR], blk64[:, :], yT[:, j, :], True, True, [cB] + yB_all, [psB])
            TT(S, "dve", tmp[0], yT[:, j, :], ps[:, 0:TBR], ALU.subtract, yB_all + [psB], [tmpB[0]])
            TT(S, "pool", tmp[1], tmp[0], tmp[0], ALU.mult, [tmpB[0]], [tmpB[1]])
            ps, psB = rps()
            MM(S, ps[:, 0:TBR], blk64[:, :], tmp[1], True, True, [cB, tmpB[1]], [psB])
            ACT(S, tmp[1], ps[:, 0:TBR], AF.Sqrt, [psB], [tmpB[1]], bias=GN_EPS)
            S.op("dve", (lambda o: (lambda e: e.reciprocal(o, o)))(tmp[1]), [tmpB[1]], [tmpB[1]])
            TT(S, "pool", tmp[0], tmp[0], tmp[1], ALU.mult, [tmpB[0], tmpB[1]], [tmpB[0]])
            ACT(S, tmp[0], tmp[0], AF.Identity, [tmpB[0], pB], [tmpB[0]], bias=vv_(j, V_GB), scale=vv_(j, V_GG))
            TT(S, "pool", tmp[0], tmp[0], bonus[:, j, :], ALU.add, [tmpB[0], boB[j]], [tmpB[0]])
            TT(S, "dve", yo[j], tmp[0], gbuf[:, j, :], ALU.mult, [tmpB[0], gbB[j]], [yoB[j]])
            ob = Buf("out")
            outBs.append(ob)
            DMA(S, "sp", D["ya_blk"](j, blk_i), yo[j], [yoB[j]], [ob])
        if "after_blk" in D:
            D["after_blk"](blk_i, outBs[-2:])
    return outBs


def consts_E():
    s = np.arange(128)[:, None]
    t = np.arange(128)[None, :]
    SU = (s < t).astype(np.float32)
    IU = (s <= t).astype(np.float32)
    SL = (t < s).astype(np.float32)
    blk = np.zeros((128, 128), np.float32)
    blk[:64, :64] = 1.0
    blk[64:, 64:] = 1.0
    return {"ident": np.eye(128, dtype=np.float32), "maskLQ": np.ascontiguousarray(np.concatenate([SU, IU], axis=1)),
            "maskSL": SL, "blk": blk}


def prep_E_weights(ab_w_in, ab_mu, rw_w0, rw_w2, rw_a0, rw_a2, rw_g2, rw_k_k, rw_k_a, rw_r_k, rw_gn_g, rw_gn_b,
                   sg_ln_g, sg_ln_b, sg_ws, sg_b, hs):
    f0, f1 = hs * 256, (hs + 1) * 256
    cols = np.concatenate([np.arange(f0, f1), 512 + np.arange(f0, f1), 1024 + np.arange(f0, f1), np.arange(1536, 1792)])
    w_rw = np.ascontiguousarray(ab_w_in[:, cols])
    mu = np.ascontiguousarray(ab_mu[cols].reshape(8, 128).T)
    w_pu = np.ascontiguousarray(ab_w_in[:, 1792:2304])
    w_pv = np.ascontiguousarray(ab_w_in[:, 2304:2816])
    w2a2 = np.ascontiguousarray(np.concatenate([rw_w2[:, f0:f1], rw_a2[:, f0:f1]], axis=0))
    g2 = np.ascontiguousarray(rw_g2[:, f0:f1])
    vs = [rw_w0, rw_a0, rw_k_k, rw_k_a, rw_r_k.reshape(-1), rw_gn_g, rw_gn_b]
    vec = np.ascontiguousarray(np.stack([v[f0:f1].reshape(2, 128).T for v in vs], axis=2)).astype(np.float32)
    wsT = np.ascontiguousarray(np.transpose(sg_ws, (0, 2, 1)))
    return dict(w_rw=w_rw, mu=mu.astype(np.float32), w_pu=w_pu, w_pv=w_pv, w2a2=w2a2, g2=g2, vec=vec, wsT=wsT,
                sg_b=np.ascontiguousarray(sg_b.reshape(-1)), sg_lng=np.ascontiguousarray(sg_ln_g.reshape(-1)),
                sg_lnb=np.ascontiguousarray(sg_ln_b.reshape(-1)))


E_SHAPES = dict(w_rw=[1024, 1024], mu=[128, 8], w_pu=[1024, 512], w_pv=[1024, 512], w2a2=[128, 256], g2=[128, 256],
                vec=[128, 2, 7], wsT=[4, 128, 128], sg_b=[512], sg_lng=[512], sg_lnb=[512])
O_SHAPES = dict(w_in=[1024, 704], nrm=[128, 4], wq=[256, HPC * 96], wq_sw=[256, HPC * 96], wkv_k=[256, HPC * 64],
                wkv_v=[256, HPC * 64])
P_SHAPES = dict(w_out=[1024, 1024], xa_wq=[1024, 1024], xa_wo=[1024, 1024], xa_wkv=[1024, 2048], lnp=[128, 6, 8],
                w_r=[1024, 36], b_r=[36], moe_wg=[32, 1024, 512], moe_wu=[32, 1024, 512], moe_wd=[32, 512, 1024])
C_SHAPES = dict(ident=[128, 128], maskLQ=[128, 256], maskSL=[128, 128], blk=[128, 128], cosT=[96, SEQ], sinT=[96, SEQ],
                masks=[128, 2048], sel=[32, 32 * 128], par=[128, 2], memT=[1024, 256], x0T=[4, 1024, 512])
PAIRS = [[0, 1], [2, 3], [4, 5], [6, 7]]


def build_fused(n_layers=4):
    nc = bass.Bass("TRN2", target_bir_lowering=False)
    X = {}

    def din(name, shape):
        X[name] = nc.dram_tensor(name, list(shape), F32, kind="ExternalInput").ap()

    for k, v in C_SHAPES.items():
        din(k, v)
    for k, v in E_SHAPES.items():
        din("E_" + k, [2] + v)
    for k, v in O_SHAPES.items():
        din("O_" + k, [2] + v)
    for k, v in P_SHAPES.items():
        din("P_" + k, [4] + v)
    outT = nc.dram_tensor("outT", [4, 1024, 512], F32, kind="ExternalOutput").ap()
    hT_pp = [nc.dram_tensor(f"hT_pp{i}", [4, 1024, 512], F32).ap() for i in range(2)]
    hb16 = nc.dram_tensor("hb16", [4, 1024, 512], BF16).ap()
    hfull16 = nc.dram_tensor("hfull16", [4, 2048, 512], BF16).ap()
    ya16 = nc.dram_tensor("ya16", [2, 256, 2048], BF16).ap()
    ya_g = nc.dram_tensor("ya_g", [2, 512, 2048], BF16).ap()
    o16 = nc.dram_tensor("o16", [4, 128, SEQ], BF16).ap()
    o_g = nc.dram_tensor("o_g", [4, 256, SEQ], BF16).ap()
    yb16 = nc.dram_tensor("yb16", [512, 2048], BF16).ap()
    with contextlib.ExitStack() as st:
        C = make_ctx(nc, st)
        S = C.S
        hb16B = bufs(4, "hb16")
        hfB = bufs(4, "hfull")
        for k in range(4):
            DMA(S, "pool", hb16[k], X["x0T"][k], [], [hb16B[k]])
        S.new_epoch()

        def ag(src, dst, reads, writes):
            S.coll(lambda e: e.collective_compute("AllGather", ALU.bypass, replica_groups=PAIRS, ins=[src], outs=[dst]),
                   reads, writes)

        for k in range(4):
            ag(hb16[k], hfull16[k], [hb16B[k]], [hfB[k]])
        outBs = []
        for layer in range(n_layers):
            j = layer // 2
            D = {n: X[n] for n in C_SHAPES}
            D["h_blk"] = lambda c, tb8: hfull16[tb8 % 4, (tb8 // 4) * 1024 + c * 128:(tb8 // 4) * 1024 + (c + 1) * 128, :]
            D["h_deps"] = (lambda hfB: (lambda tb8: [hfB[tb8 % 4]]))(hfB)
            if layer % 2 == 0:
                for n in E_SHAPES:
                    D[n] = X["E_" + n][j]
                D["hsg_blk"] = lambda c, tb: hb16[tb, c * 128:(c + 1) * 128, :]
                D["ya_blk"] = lambda jj, bi: ya16[bi // 8, jj * 128:(jj + 1) * 128, (bi % 8) * TBR:(bi % 8 + 1) * TBR]
                D["yb_blk"] = lambda ci: yb16.rearrange("(g p) t -> p g t", p=128)[:, :, ci * 128:(ci + 1) * 128]
                xB = bufs(2, "yag")
                acc = []

                def after_blk(bi, bs, xB=xB, acc=acc):
                    acc.extend(bs)
                    if bi % 8 == 7:
                        ag(ya16[bi // 8], ya_g[bi // 8], list(acc), [xB[bi // 8]])
                        del acc[:]
                D["after_blk"] = after_blk
                stage_E(C, D)
                S.new_epoch()
                mix_src = []
                for c in range(8):
                    if c < 4:
                        r, q = c // 2, c % 2
                        mix_src.append(("xchg", (lambda r, q: (lambda tb, half: ya_g[half, r * 256 + q * 128:r * 256 + (q + 1) * 128,
                                                 tb * 512:(tb + 1) * 512]))(r, q), [xB[0], xB[1]]))
                    else:
                        mix_src.append(("local", (lambda c: (lambda tb: yb16[(c - 4) * 128:(c - 3) * 128,
                                                  tb * 512:(tb + 1) * 512]))(c), []))
            else:
                for n in O_SHAPES:
                    D[n] = X["O_" + n][j]
                D["o_blk"] = lambda h, qb: o16[h // 2, (h % 2) * 64:(h % 2) * 64 + 64, qb * 512:(qb + 1) * 512]
                xB = bufs(4, "og")
                acc = []

                def after_head(h, bs, xB=xB, acc=acc):
                    acc.extend(bs)
                    if h % 2 == 1:
                        ag(o16[h // 2], o_g[h // 2], list(acc), [xB[h // 2]])
                        del acc[:]
                D["after_head"] = after_head
                stage_O(C, D)
                S.new_epoch()
                mix_src = []
                for c in range(8):
                    r, q = c // 4, c % 4
                    mix_src.append(("xchg", (lambda r, q: (lambda tb, half: o_g[q, r * 128:(r + 1) * 128,
                                             half * 2048 + tb * 512: half * 2048 + (tb + 1) * 512]))(r, q), [xB[q]]))
            D = {n: X[n] for n in C_SHAPES}
            for n in P_SHAPES:
                D[n] = X["P_" + n][layer]
            h_in = X["x0T"] if layer == 0 else hT_pp[layer % 2]
            h_out = outT if layer == n_layers - 1 else hT_pp[(layer + 1) % 2]
            D["h_blk"] = (lambda h_in: (lambda c, tb: h_in[tb, c * 128:(c + 1) * 128, :]))(h_in)
            D["out_blk"] = (lambda h_out: (lambda c, tb: h_out[tb, c * 128:(c + 1) * 128, :]))(h_out)
            if layer != n_layers - 1:
                D["out16_blk"] = lambda c, tb: hb16[tb, c * 128:(c + 1) * 128, :]
                hfB = bufs(4, "hfull")
                D["after_tb"] = (lambda hfB: (lambda tb, bs: ag(hb16[tb], hfull16[tb], list(bs), [hfB[tb]])))(hfB)
            D["mix_src"] = mix_src
            outBs = stage_P(C, D)
            if layer != n_layers - 1:
                S.new_epoch()
        S.final_wait("sp", outBs)
        S.run()
    return nc


_PROG = {}


def kernel(**inp):
    inp = {k: np.asarray(v) for k, v in inp.items()}
    x, mem = inp["x"], inp["mem"]
    half = SEQ // 2
    if "nc" not in _PROG:
        _PROG["nc"] = build_fused()
    nc = _PROG["nc"]
    cst = {}
    cst.update(consts_E()); cst.update(consts_O()); cst.update(consts_P())
    e_names = ["ab_w_in", "ab_mu", "rw_w0", "rw_w2", "rw_a0", "rw_a2", "rw_g2", "rw_k_k", "rw_k_a", "rw_r_k",
               "rw_gn_g", "rw_gn_b", "sg_ln_g", "sg_ln_b", "sg_ws", "sg_b"]
    shared = {}
    w_out4 = np.stack([inp["ab_w_out"][0], inp["mla_w_out"][0], inp["ab_w_out"][1], inp["mla_w_out"][1]])
    shared["P_w_out"] = np.ascontiguousarray(w_out4)
    shared["P_xa_wq"] = inp["xa_wq"]; shared["P_xa_wo"] = inp["xa_wo"]; shared["P_xa_wkv"] = inp["xa_wkv"]
    shared["P_lnp"] = np.stack([lnp_layout([inp["ln1_g"][l], inp["ln1_b"][l], inp["ln2_g"][l], inp["ln2_b"][l],
                                            inp["ln3_g"][l], inp["ln3_b"][l]]) for l in range(4)])
    shared["P_w_r"] = np.ascontiguousarray(np.concatenate([inp["moe_w_group"], inp["moe_w_expert"]], axis=2))
    shared["P_b_r"] = np.ascontiguousarray(np.concatenate([inp["moe_b_group"], inp["moe_b_expert"]], axis=1))
    shared["P_moe_wg"] = inp["moe_w_gate"]; shared["P_moe_wu"] = inp["moe_w_up"]; shared["P_moe_wd"] = inp["moe_w_down"]
    per_hs = []
    for hs in range(2):
        d = {}
        ew = [prep_E_weights(*[inp[n][j] for n in e_names], hs) for j in range(2)]
        for n in E_SHAPES:
            d["E_" + n] = np.ascontiguousarray(np.stack([ew[j][n] for j in range(2)]))
        ow = [prep_O_weights(inp["mla_w_in"][j], inp["mla_q_norm"][j], inp["mla_kv_norm"][j], inp["mla_wq_b"][j],
                             inp["mla_wkv_b"][j], hs) for j in range(2)]
        for n in O_SHAPES:
            d["O_" + n] = np.ascontiguousarray(np.stack([ow[j][n] for j in range(2)]))
        par = np.zeros((128, 2), np.float32)
        par[:, hs] = 1.0
        d["par"] = par
        per_hs.append(d)
    in_maps = []
    for c in range(NCORES):
        b, s = c // 2, c % 2
        m = dict(cst)
        m.update(shared)
        m.update(per_hs[s])
        xT = x[b, s * half:(s + 1) * half].T
        m["x0T"] = np.ascontiguousarray(xT.reshape(1024, 4, 512).transpose(1, 0, 2))
        m["memT"] = np.ascontiguousarray(mem[b].T)
        in_maps.append({k: np.ascontiguousarray(v, dtype=np.float32) for k, v in m.items()})
    res = run_bass_kernel_spmd(nc, in_maps, core_ids=list(range(NCORES)))
    out = np.empty((x.shape[0], SEQ, 1024), np.float32)
    for c in range(NCORES):
        b, s = c // 2, c % 2
        o = res.results[c]["outT"]
        out[b, s * half:(s + 1) * half, :] = o.transpose(0, 2, 1).reshape(half, 1024)
    return out
```
